# Optimizing a Trainium2 kernel written in Bass

```python
import math
import jax, jax.numpy as jnp
from jax import lax
import numpy as np

D_MODEL = 1024
BATCH = 16
SEQ = 2048
DEPTH = 1

POOL_GROUPS = 4
POOL_WINDOWS = (2, 4, 8, 16)
POOL_GROUP_DIM = D_MODEL // 8
POOL_WIDTH = POOL_GROUPS * POOL_GROUP_DIM
ATTN_HEADS = 8
HEAD_DIM = D_MODEL // 16
ATTN_WIDTH = ATTN_HEADS * HEAD_DIM
MOBA_BLOCK = 256
MOBA_TOPK = 3
Q_CHUNK = 128
REL_BUCKETS = 32
REL_MAX_DIST = 128
MEM_LEN = 256
MEM_HEADS = 4
MEM_HEAD_DIM = D_MODEL // 8
MEM_WIDTH = MEM_HEADS * MEM_HEAD_DIM
MOE_GROUPS = 4
MOE_EXPERTS_PER_GROUP = 8
MOE_EXPERTS = MOE_GROUPS * MOE_EXPERTS_PER_GROUP
MOE_TOPK = 2
EXPERT_FF = D_MODEL // 4
N_BRANCH = 2
IN_WIDTH = POOL_WIDTH + 3 * ATTN_WIDTH + N_BRANCH * D_MODEL
DN_ALPHA = (2.0 * DEPTH) ** 0.25
DN_BETA = (8.0 * DEPTH) ** -0.25
LN_EPS = 1e-5

kernel_name = "hybrid_pool_moba_hmoe_deepnorm"


def layer_norm(x, g, b):
    xf = x.astype(jnp.float32)
    mu = jnp.mean(xf, axis=-1, keepdims=True)
    var = jnp.mean(jnp.square(xf - mu), axis=-1, keepdims=True)
    return ((xf - mu) * lax.rsqrt(var + LN_EPS)).astype(x.dtype) * g + b


def rel_bucket(dist):
    max_exact = REL_BUCKETS // 2
    d = jnp.maximum(dist, 0)
    large = max_exact + (jnp.log(jnp.maximum(d, 1).astype(jnp.float32) / max_exact)
                         / math.log(REL_MAX_DIST / max_exact) * (REL_BUCKETS - max_exact)).astype(jnp.int32)
    large = jnp.minimum(large, REL_BUCKETS - 1)
    return jnp.where(d < max_exact, d, large)


def pool_mixer(u, w_grp, scale):
    B, S, C = u.shape
    win = jnp.repeat(jnp.array(POOL_WINDOWS, jnp.int32), POOL_GROUP_DIM)
    uf = u.astype(jnp.float32)
    cs = jnp.pad(jnp.cumsum(uf, axis=1), ((0, 0), (1, 0), (0, 0)))
    t = jnp.arange(S, dtype=jnp.int32)[:, None]
    lo = jnp.maximum(t + 1 - win[None, :], 0)
    win_sum = cs[:, 1:, :] - cs[:, lo, jnp.arange(C)[None, :]]
    count = jnp.minimum(t + 1, win[None, :]).astype(jnp.float32)
    y = (win_sum / count - uf).astype(u.dtype)
    y = jnp.einsum('bsgc,gce->bsge', y.reshape(B, S, POOL_GROUPS, POOL_GROUP_DIM), w_grp)
    return y.reshape(B, S, C) * scale


def moba_attention(q, k, v, rel_table):
    B, S, H, Dh = q.shape
    nb = -(-S // MOBA_BLOCK)
    sp = nb * MOBA_BLOCK
    pad = ((0, 0), (0, sp - S), (0, 0), (0, 0))
    q, k, v = [jnp.pad(a, pad).transpose(0, 2, 1, 3) for a in (q, k, v)]
    kb = k.reshape(B, H, nb, MOBA_BLOCK, Dh)
    vb = v.reshape(B, H, nb, MOBA_BLOCK, Dh)
    n_sel = min(MOBA_TOPK, nb - 1)
    qblk = jnp.arange(sp, dtype=jnp.int32) // MOBA_BLOCK
    if n_sel > 0:
        kbar = jnp.mean(kb.astype(jnp.float32), axis=3)
        gate = jnp.einsum('bhsd,bhnd->bhsn', q.astype(jnp.float32), kbar)
        past = jnp.arange(nb, dtype=jnp.int32)[None, :] < qblk[:, None]
        gate = jnp.where(past, gate, -jnp.inf)
        _, sel = lax.top_k(gate, n_sel)
        sel = sel.astype(jnp.int32)
    else:
        sel = jnp.zeros((B, H, sp, 0), jnp.int32)
    table_h = rel_table.T
    scale = HEAD_DIM ** -0.5
    n_chunks = sp // Q_CHUNK
    hidx = jnp.arange(H)[:, None, None]
    l_idx = jnp.arange(MOBA_BLOCK, dtype=jnp.int32)

    def one_batch(args):
        qb_, kb_, vb_, sel_ = args

        def one_chunk(c):
            start = c * Q_CHUNK
            qc = lax.dynamic_slice_in_dim(qb_, start, Q_CHUNK, axis=1)
            sc = lax.dynamic_slice_in_dim(sel_, start, Q_CHUNK, axis=1)
            tq = start + jnp.arange(Q_CHUNK, dtype=jnp.int32)
            blk = start // MOBA_BLOCK
            ks = kb_[hidx, sc]
            vs = vb_[hidx, sc]
            kpos_s = sc[..., None] * MOBA_BLOCK + l_idx
            bias_s = table_h[hidx[..., None], rel_bucket(tq[None, :, None, None] - kpos_s)]
            ls = jnp.einsum('hqd,hqnld->hqnl', qc, ks).astype(jnp.float32) * scale + bias_s
            ls = jnp.where((sc < blk)[..., None], ls, -jnp.inf).reshape(H, Q_CHUNK, n_sel * MOBA_BLOCK)
            ko = lax.dynamic_index_in_dim(kb_, blk, axis=1, keepdims=False)
            vo = lax.dynamic_index_in_dim(vb_, blk, axis=1, keepdims=False)
            dist_o = tq[:, None] - (blk * MOBA_BLOCK + l_idx)[None, :]
            bias_o = table_h[:, rel_bucket(dist_o)]
            lo = jnp.einsum('hqd,hld->hql', qc, ko).astype(jnp.float32) * scale + bias_o
            lo = jnp.where(dist_o[None] >= 0, lo, -jnp.inf)
            p = jax.nn.softmax(jnp.concatenate([ls, lo], axis=-1), axis=-1).astype(qc.dtype)
            ps = p[..., :n_sel * MOBA_BLOCK].reshape(H, Q_CHUNK, n_sel, MOBA_BLOCK)
            po = p[..., n_sel * MOBA_BLOCK:]
            return jnp.einsum('hqnl,hqnld->hqd', ps, vs) + jnp.einsum('hql,hld->hqd', po, vo)

        outs = lax.map(one_chunk, jnp.arange(n_chunks, dtype=jnp.int32))
        return outs.transpose(1, 0, 2, 3).reshape(H, sp, Dh)

    out = lax.map(one_batch, (q, kb, vb, sel))
    return out.transpose(0, 2, 1, 3)[:, :S].reshape(B, S, H * Dh)


def hybrid_mixer(h, w_in, b_gate, w_pool_grp, pool_scale, w_pool_up, w_attn_up, w_out, rel_table):
    B, S, _ = h.shape
    z = h @ w_in
    P, A = POOL_WIDTH, ATTN_WIDTH
    u, q, k, v, gl = jnp.split(z, [P, P + A, P + 2 * A, P + 3 * A], axis=-1)
    gates = jax.nn.sigmoid(gl + b_gate).reshape(B, S, N_BRANCH, D_MODEL)
    y_pool = pool_mixer(u, w_pool_grp, pool_scale) @ w_pool_up
    hd = (B, S, ATTN_HEADS, HEAD_DIM)
    y_attn = moba_attention(q.reshape(hd), k.reshape(hd), v.reshape(hd), rel_table) @ w_attn_up
    merged = gates[:, :, 0] * y_pool + gates[:, :, 1] * y_attn
    return merged @ w_out


def memory_attention(h, mem, wq, wk, wv, wo):
    B, S, _ = h.shape
    M = mem.shape[1]
    q = (h @ wq).reshape(B, S, MEM_HEADS, MEM_HEAD_DIM)
    k = (mem @ wk).reshape(B, M, MEM_HEADS, MEM_HEAD_DIM)
    v = (mem @ wv).reshape(B, M, MEM_HEADS, MEM_HEAD_DIM)
    logits = jnp.einsum('bshd,bmhd->bhsm', q, k).astype(jnp.float32) * (MEM_HEAD_DIM ** -0.5)
    p = jax.nn.softmax(logits, axis=-1).astype(h.dtype)
    o = jnp.einsum('bhsm,bmhd->bshd', p, v).reshape(B, S, MEM_WIDTH)
    return o @ wo


def hier_moe(h, w_coarse, b_coarse, w_fine, b_fine, w_gate, w_up, w_down):
    B, S, _ = h.shape
    hf = h.astype(jnp.float32)
    coarse = jax.nn.softmax(jnp.einsum('bsd,dg->bsg', hf, w_coarse) + b_coarse, axis=-1)
    g_prob, g_idx = lax.top_k(coarse, 1)
    fine_logits = jnp.einsum('bsd,dge->bsge', hf, w_fine) + b_fine
    fine_sel = jnp.take_along_axis(fine_logits, g_idx[..., None], axis=2)[:, :, 0]
    e_prob, e_idx = lax.top_k(jax.nn.softmax(fine_sel, axis=-1), MOE_TOPK)
    e_prob = e_prob / jnp.sum(e_prob, axis=-1, keepdims=True)
    w_within = jnp.sum(jax.nn.one_hot(e_idx, MOE_EXPERTS_PER_GROUP) * e_prob[..., None], axis=-2)
    combine = jax.nn.one_hot(g_idx[..., 0], MOE_GROUPS)[..., None] * (g_prob * w_within)[..., None, :]
    combine = combine.reshape(B, S, MOE_EXPERTS).astype(h.dtype)

    def per_row(args):
        xr, cr = args
        a = jnp.einsum('sd,edf->sef', xr, w_gate)
        b = jnp.einsum('sd,edf->sef', xr, w_up)
        hid = jax.nn.silu(a) * b * cr[..., None]
        return jnp.einsum('sef,efd->sd', hid, w_down)

    return lax.map(per_row, (h, combine))


def setup_inputs(seed: int = 0) -> dict:
    key = jax.random.key(seed)
    ks = iter(jax.random.split(key, 40))
    f32 = jnp.float32
    L, D = DEPTH, D_MODEL

    def nrm(shape, s):
        return jax.random.normal(next(ks), shape, f32) * s

    def gain(shape):
        return 1.0 + nrm(shape, 0.02)

    x = nrm((BATCH, SEQ, D), 1.0)
    mem = nrm((BATCH, MEM_LEN, D), 1.0)
    col_scale = jnp.concatenate([
        jnp.ones((POOL_WIDTH + 2 * ATTN_WIDTH,), f32),
        jnp.full((ATTN_WIDTH,), DN_BETA, f32),
        jnp.ones((N_BRANCH * D,), f32)])
    w_in = nrm((L, D, IN_WIDTH), D ** -0.5) * col_scale
    return {
        "x": x,
        "mem": mem,
        "ln_in_g": gain((D,)),
        "ln_in_b": nrm((D,), 0.02),
        "rel_bias": nrm((REL_BUCKETS, ATTN_HEADS), 0.2),
        "w_in": w_in,
        "b_gate": nrm((L, N_BRANCH * D), 0.02),
        "w_pool_grp": nrm((L, POOL_GROUPS, POOL_GROUP_DIM, POOL_GROUP_DIM), POOL_GROUP_DIM ** -0.5),
        "pool_scale": 1.0 + nrm((L, POOL_WIDTH), 0.1),
        "w_pool_up": nrm((L, POOL_WIDTH, D), POOL_WIDTH ** -0.5),
        "w_attn_up": nrm((L, ATTN_WIDTH, D), ATTN_WIDTH ** -0.5),
        "w_mix_out": nrm((L, D, D), D ** -0.5 * DN_BETA),
        "ln1_g": gain((L, D)),
        "ln1_b": nrm((L, D), 0.02),
        "w_mq": nrm((L, D, MEM_WIDTH), D ** -0.5),
        "w_mk": nrm((L, D, MEM_WIDTH), D ** -0.5),
        "w_mv": nrm((L, D, MEM_WIDTH), D ** -0.5 * DN_BETA),
        "w_mo": nrm((L, MEM_WIDTH, D), MEM_WIDTH ** -0.5 * DN_BETA),
        "ln2_g": gain((L, D)),
        "ln2_b": nrm((L, D), 0.02),
        "w_coarse": nrm((L, D, MOE_GROUPS), D ** -0.5),
        "b_coarse": nrm((L, MOE_GROUPS), 0.01),
        "w_fine": nrm((L, D, MOE_GROUPS, MOE_EXPERTS_PER_GROUP), D ** -0.5),
        "b_fine": nrm((L, MOE_GROUPS, MOE_EXPERTS_PER_GROUP), 0.01),
        "w_gate": nrm((L, MOE_EXPERTS, D, EXPERT_FF), D ** -0.5),
        "w_up": nrm((L, MOE_EXPERTS, D, EXPERT_FF), D ** -0.5),
        "w_down": nrm((L, MOE_EXPERTS, EXPERT_FF, D), EXPERT_FF ** -0.5 * DN_BETA),
        "ln3_g": gain((L, D)),
        "ln3_b": nrm((L, D), 0.02),
    }


def reference(x, mem, ln_in_g, ln_in_b, rel_bias, w_in, b_gate, w_pool_grp, pool_scale, w_pool_up, w_attn_up,
              w_mix_out, ln1_g, ln1_b, w_mq, w_mk, w_mv, w_mo, ln2_g, ln2_b, w_coarse, b_coarse, w_fine, b_fine,
              w_gate, w_up, w_down, ln3_g, ln3_b):
    h = layer_norm(x, ln_in_g, ln_in_b)
    for l in range(DEPTH):
        mix = hybrid_mixer(h, w_in[l], b_gate[l], w_pool_grp[l], pool_scale[l], w_pool_up[l], w_attn_up[l],
                           w_mix_out[l], rel_bias)
        h = layer_norm(DN_ALPHA * h + mix, ln1_g[l], ln1_b[l])
        xa = memory_attention(h, mem, w_mq[l], w_mk[l], w_mv[l], w_mo[l])
        h = layer_norm(DN_ALPHA * h + xa, ln2_g[l], ln2_b[l])
        ff = hier_moe(h, w_coarse[l], b_coarse[l], w_fine[l], b_fine[l], w_gate[l], w_up[l], w_down[l])
        h = layer_norm(DN_ALPHA * h + ff, ln3_g[l], ln3_b[l])
    return h
```

```python
import math
import numpy as np
import ml_dtypes
import concourse.bass as bass
import concourse.mybir as mybir
from concourse.bass_utils import run_bass_kernel_spmd

F32 = mybir.dt.float32
BF16 = mybir.dt.bfloat16
AF = mybir.ActivationFunctionType
ALU = mybir.AluOpType
AX = mybir.AxisListType

D = 1024
S = 2048
NB = 2
NCORES = 8
KC = D // 128
NT = S // 128
NQB = S // 512
H = 8
DH = 64
MEMLEN = 256
MH = 4
NE = 32
FF = 256
ALPHA = 2.0 ** 0.25
EPS = 1e-5
NEG = -30000.0


class _Op:
    __slots__ = ("eng", "fn", "deps", "idx", "sig", "sigval", "dma_key", "dma_val", "waits")


class Sched:
    def __init__(self, same_engine_sync=True):
        self.ops = []
        self.lastw = {}
        self.readers = {}
        self.dma_count = {}
        self.same_engine_sync = same_engine_sync

    def add(self, eng, meth, kw, reads=(), writes=(), dma_key=None):
        op = _Op()
        op.eng, op.fn, op.idx, op.dma_key = eng, (meth, kw), len(self.ops), dma_key
        op.sig, op.sigval, op.dma_val, op.waits = False, 0, 0, []
        deps = set()
        for t in reads:
            w = self.lastw.get(t)
            if w is not None:
                deps.add(w)
        for t in writes:
            w = self.lastw.get(t)
            if w is not None:
                deps.add(w)
            for r in self.readers.get(t, ()):
                deps.add(r)
        deps.discard(op.idx)
        op.deps = deps
        for t in reads:
            self.readers.setdefault(t, []).append(op.idx)
        for t in writes:
            self.lastw[t] = op.idx
            self.readers[t] = []
        if dma_key is not None:
            self.dma_count[dma_key] = self.dma_count.get(dma_key, 0) + 1
            op.dma_val = 16 * self.dma_count[dma_key]
        self.ops.append(op)
        return op.idx

    def barrier(self, engines=("pe", "act", "dve", "pool", "sp")):
        last = {}
        for op in self.ops:
            if op.dma_key is not None:
                last[("dma", op.dma_key)] = op.idx
            else:
                last[("eng", op.eng)] = op.idx
        deps = set(last.values())
        for e in engines:
            op = _Op()
            op.eng, op.fn, op.idx, op.dma_key = e, ("nop", {}), len(self.ops), None
            op.sig, op.sigval, op.dma_val, op.waits = False, 0, 0, []
            op.deps = set(deps)
            self.ops.append(op)

    def finalize(self):
        ops = self.ops
        for op in ops:
            best = {}
            for d in op.deps:
                p = ops[d]
                if p.dma_key is not None:
                    continue
                if p.eng == op.eng and (p.eng == "pe" or not self.same_engine_sync) and op.dma_key is None:
                    continue
                if d > best.get(p.eng, -1):
                    best[p.eng] = d
            for d in best.values():
                ops[d].sig = True
        cnt = {}
        for op in ops:
            if op.sig:
                cnt[op.eng] = cnt.get(op.eng, 0) + 1
                op.sigval = cnt[op.eng]
        waited = {}
        for op in ops:
            need = {}
            for d in op.deps:
                p = ops[d]
                if p.dma_key is not None:
                    k, v = ("dma", p.dma_key), p.dma_val
                else:
                    if not p.sig:
                        continue
                    if p.eng == op.eng and (p.eng == "pe" or not self.same_engine_sync) and op.dma_key is None:
                        continue
                    k, v = ("eng", p.eng), p.sigval
                if v > need.get(k, 0):
                    need[k] = v
            w = waited.setdefault(op.eng, {})
            for k, v in need.items():
                if v > w.get(k, 0):
                    w[k] = v
                    op.waits.append((k, v))

    def emit(self, nc, eng_name, eng, sems):
        n = 0
        for op in self.ops:
            if op.eng != eng_name:
                continue
            for k, v in op.waits:
                eng.wait_ge(sems[k], v)
            ins = getattr(eng, op.fn[0])(**op.fn[1])
            if op.dma_key is not None:
                ins.then_inc(sems[("dma", op.dma_key)], 16)
            elif op.sig:
                ins.then_inc(sems[("eng", op.eng)], 1)
            n += 1
        return n


class Arena:
    def __init__(self, nc, start=16640, limit=224 * 1024):
        self.nc, self.off, self.limit, self.n = nc, start, limit, 0
        self.peak = 0

    def alloc(self, name, shape, dtype):
        nbytes = int(np.prod(shape[1:])) * (4 if dtype == F32 else 2)
        nbytes = (nbytes + 63) // 64 * 64
        self.n += 1
        t = self.nc.alloc_sbuf_tensor_at(f"{name}_{self.n}", list(shape), dtype, offset=self.off)
        self.off += nbytes
        self.peak = max(self.peak, self.off)
        assert self.off <= self.limit, f"SBUF arena overflow at {name}: {self.off}"
        return t

    def mark(self):
        return self.off

    def release(self, m):
        self.off = m


class SegArena:
    _n = [0]

    def __init__(self, nc, segs):
        self.nc = nc
        self.segs = [[a, a, a + sz] for a, sz in segs]

    def alloc(self, name, shape, dtype):
        nbytes = int(np.prod(shape[1:])) * (4 if dtype == F32 else 2)
        nbytes = (nbytes + 63) // 64 * 64
        for sg in self.segs:
            if sg[1] + nbytes <= sg[2]:
                SegArena._n[0] += 1
                t = self.nc.alloc_sbuf_tensor_at(f"{name}_s{SegArena._n[0]}", list(shape), dtype, offset=sg[1])
                sg[1] += nbytes
                return t
        raise AssertionError(f"SegArena overflow at {name} ({nbytes} B); segs={self.segs}")


def _bf16(a):
    return np.asarray(a, dtype=np.float32).astype(ml_dtypes.bfloat16)


STAGES = ["A0", "POOL", "ATT", "A3", "B1", "B2", "all"]


def build_program(stop_after="all", nb=NB, debug=True):
    nc = bass.Bass("TRN2", target_bir_lowering=False)
    sc = Sched()
    stage_i = STAGES.index(stop_after)
    dbg_out = []

    def din(name, shape, dt=F32):
        return nc.dram_tensor(name, list(shape), dt, kind="ExternalInput").ap()

    x_d = din("x", [nb, S, D])
    mem_d = din("mem", [nb, MEMLEN, D])
    lnp_d = din("lnp", [8, D])
    ident_d = din("ident", [128, 128], BF16)
    identf_d = din("identf", [128, 128])
    shiftb_d = din("shiftb", [64, 128])
    w_in_d = din("w_in", [D, 4096])
    bgate_d = din("b_gate", [2048])
    wpg_d = din("w_pool_grp", [4, 128, 128])
    pscale_d = din("pool_scale", [512])
    wpu_d = din("w_pool_up", [512, D])
    wau_d = din("w_attn_up", [512, D])
    wout_d = din("w_mix_out", [D, D])
    wmq_d = din("w_mq", [D, 512])
    wmk_d = din("w_mk", [D, 512])
    wmv_d = din("w_mv", [D, 512])
    wmo_d = din("w_mo", [512, D])
    wr_d = din("w_router", [D, 36])
    br_d = din("b_router", [36])
    wg_d = din("w_gate", [NE, D, FF])
    wu_d = din("w_up", [NE, D, FF])
    wd_d = din("w_down", [NE, FF, D])
    biasT_d = din("biasT", [H, 2, 128, 128])
    b31_d = din("b31", [128, H])
    maskd_d = din("maskd", [128, 128])
    pastb_d = din("pastbias", [128, NT, 8])
    ownfix_d = din("ownfix", [128, NT, 8])
    ind_d = din("indrows", [8, S], BF16)
    invc_d = din("invc", [128, 4, 16])
    out_d = nc.dram_tensor("out", [nb, S, D], F32, kind="ExternalOutput").ap()

    A = Arena(nc)
    P = [nc.alloc_psum_tensor(f"psb{i}", [128, 1024], F32) for i in range(4)]

    def bank(i):
        return P[i // 2][:, (i % 2) * 512:(i % 2 + 1) * 512]

    def PT(i):
        return ("ps", i)

    def dma(q, out, in_, reads, writes, key, **kw):
        sc.add(q, "dma_start", dict(out=out, in_=in_, **kw), reads, writes, dma_key=key)

    def mm(out, lhsT, rhs, start, stop, reads, writes):
        sc.add("pe", "matmul", dict(out=out, lhsT=lhsT, rhs=rhs, start=start, stop=stop), reads, writes)

    def tr(out, in_, reads, writes):
        sc.add("pe", "transpose", dict(out=out, in_=in_, identity=ident[:, :]), list(reads) + [("ident",)], writes)

    def act(out, in_, func, reads, writes, bias=0.0, scale=1.0):
        sc.add("act", "activation", dict(out=out, in_=in_, func=func, bias=bias, scale=scale), reads, writes)

    def tt(eng, out, in0, in1, op, reads, writes):
        sc.add(eng, "tensor_tensor", dict(out=out, in0=in0, in1=in1, op=op), reads, writes)

    def ts(eng, out, in0, s1, s2, op0, op1, reads, writes):
        kw = dict(out=out, in0=in0, scalar1=s1, scalar2=s2, op0=op0)
        if op1 is not None:
            kw["op1"] = op1
        sc.add(eng, "tensor_scalar", kw, reads, writes)

    def stt(eng, out, in0, scalar, in1, op0, op1, reads, writes):
        sc.add(eng, "scalar_tensor_tensor", dict(out=out, in0=in0, scalar=scalar, in1=in1, op0=op0, op1=op1), reads, writes)

    def cp(eng, out, in_, reads, writes):
        if eng == "act":
            sc.add("act", "copy", dict(out=out, in_=in_), reads, writes)
        else:
            sc.add(eng, "tensor_copy", dict(out=out, in_=in_), reads, writes)

    def memset(eng, ap, val, writes):
        sc.add(eng, "memset", dict(ap=ap, constant=val), (), writes)

    def dump(name, ap, shape, dt, reads):
        if not debug:
            return
        d = nc.dram_tensor(name, list(shape), dt, kind="ExternalOutput").ap()
        dbg_out.append(name)
        dma("sp", d, ap, reads, [("dbg", name)], "dbg_" + name)

    ident = A.alloc("ident", [128, 128], BF16)
    identf = A.alloc("identf", [128, 128], F32)
    shiftb = A.alloc("shiftb", [64, 128], F32)
    lncol = A.alloc("lncol", [128, 8, KC], F32)
    epsc = A.alloc("epsc", [128, 1], F32)
    onesb = A.alloc("onesb", [128, 128], BF16)
    dma("sp", ident[:, :], ident_d[:, :], [], [("ident",)], "c0_6")
    dma("sp", identf[:, :], identf_d[:, :], [], [("identf",)], "c0_7")
    dma("sp", shiftb[:, :], shiftb_d[:, :], [], [("shiftb",)], "c0_8")
    dma("sp", lncol[:, :, :], lnp_d.rearrange("r (k p) -> p r k", p=128), [], [("lncol",)], "c0_lncol",
        allow_slow_non_contiguous=True)
    memset("pool", epsc[:, :], EPS, [("epsc",)])
    memset("pool", onesb[:, :], 1.0, [("onesb",)])

    def load_lnbc(dst, r0):
        dma("sp", dst[:, :, :], lnp_d[r0:r0 + 2, :].partition_broadcast(128), [], [("lnbc", r0)], f"lnbc{r0}")

    def layer_norm_stats(src_ap, src_tok, st_t, mv_t, tag):
        for g in range(2):
            sc.add("dve", "bn_stats", dict(out=st_t[:, g, :], in_=src_ap[:, g * 512:(g + 1) * 512]), [src_tok], [("st", tag)])
        sc.add("dve", "bn_aggr", dict(out=mv_t[:, 0:2], in_=st_t[:, :, :].rearrange("p g n -> p (g n)")), [("st", tag)], [("mv", tag)])
        act(mv_t[:, 2:3], mv_t[:, 1:2], AF.Sqrt, [("mv", tag), ("epsc",)], [("mv", tag)], bias=epsc[:, 0:1], scale=1.0)
        sc.add("dve", "reciprocal", dict(out=mv_t[:, 2:3], in_=mv_t[:, 2:3]), [("mv", tag)], [("mv", tag)])
        stt("dve", mv_t[:, 3:4], mv_t[:, 0:1], -1.0, mv_t[:, 2:3], ALU.mult, ALU.mult, [("mv", tag)], [("mv", tag)])

    def transposes_to_T(xn4, dstT, blk, grow, brow, xn_tok, dst_name):
        for kc in range(KC):
            bi = kc % 2
            pb = bank(bi).bitcast(BF16)
            for j in range(4):
                tr(pb[:, j * 128:(j + 1) * 128], xn4[j][:, kc * 128:(kc + 1) * 128], [(xn_tok, j)], [PT(bi)])
            act(dstT[:, kc, blk * 512:(blk + 1) * 512], pb[:, 0:512], AF.Identity, [PT(bi), ("lncol",)],
                [(dst_name, kc, blk)], bias=lncol[:, brow, kc:kc + 1], scale=lncol[:, grow, kc:kc + 1])

    WKC = "(kc p) n -> p kc n"

    acc = A.alloc("acc", [128, NT, D], F32)
    hT = A.alloc("hT", [128, KC, S], BF16)
    R_hT = (A.off - 32768, 32768)
    mT = A.alloc("mT", [128, KC, S], BF16)
    R_m = (A.off - 32768, 32768)
    ypg = A.alloc("ypg", [128, 4, S], BF16)
    R_x = (A.off - 16384, 16384)
    attnT = A.alloc("attnT", [128, 4, S], BF16)
    R_y = (A.off - 16384, 16384)
    R_tmp = (A.off, A.limit - A.off)

    class _Ph:
        def __init__(self):
            self.cur = None
        def phase(self, segs):
            self.cur = SegArena(nc, segs)
        def alloc(self, name, shape, dtype):
            return self.cur.alloc(name, shape, dtype)
        def mark(self):
            return 0
        def release(self, m):
            pass
    A = _Ph()

    for b in range(nb):
        mk_b = 0
        A.phase([R_tmp, R_m, R_x, R_y])

        mk = A.mark()
        lnbc = A.alloc("lnbc", [128, 2, D], F32)
        load_lnbc(lnbc, 0)
        xt8 = [A.alloc("xt", [128, D], F32) for _ in range(8)]
        xn = [A.alloc("xn", [128, D], BF16) for _ in range(4)]
        st = [A.alloc("st", [128, 2, 6], F32) for _ in range(4)]
        mv = [A.alloc("mv", [128, 4], F32) for _ in range(4)]
        for t in range(NT):
            s4, blk = t % 4, t // 4
            s8 = t % 8
            xt = {s4: xt8[s8]}
            dma("sp", xt[s4][:, :], x_d[b, t * 128:(t + 1) * 128, :], [], [("xt", s8)], f"xt{s8}")
            layer_norm_stats(xt[s4], ("xt", s8), st[s4], mv[s4], s4)
            act(xn[s4][:, :], xt[s4][:, :], AF.Identity, [("xt", s8), ("mv", s4)], [("xn", s4)],
                bias=mv[s4][:, 3:4], scale=mv[s4][:, 2:3])
            act(acc[:, t, :], xt[s4][:, :], AF.Identity, [("xt", s8), ("mv", s4)], [("acc", t)],
                bias=mv[s4][:, 3:4], scale=mv[s4][:, 2:3])
            tt("pool", acc[:, t, :], acc[:, t, :], lnbc[:, 0, :], ALU.mult, [("acc", t), ("lnbc", 0)], [("acc", t)])
            if t >= 1:
                tt("dve", acc[:, t - 1, :], acc[:, t - 1, :], lnbc[:, 1, :], ALU.add, [("acc", t - 1), ("lnbc", 0)], [("acc", t - 1)])
            if s4 == 3:
                transposes_to_T(xn, hT, blk, 0, 1, "xn", "hT")
        tt("dve", acc[:, NT - 1, :], acc[:, NT - 1, :], lnbc[:, 1, :], ALU.add, [("acc", NT - 1), ("lnbc", 0)], [("acc", NT - 1)])
        A.release(mk)
        sc.barrier()
        hT_all = [("hT", kc, blk) for kc in range(KC) for blk in range(NQB)]
        if stage_i == 0:
            if b == 0:
                dump("dbg_hT", hT[:, :, :], [128, KC, S], BF16, hT_all)
            for t in range(NT):
                dma("sp", out_d[b, t * 128:(t + 1) * 128, :], acc[:, t, :], [("acc", t)], [("outdram", b, t)], "out")
            A.release(mk_b)
            sc.barrier()
            continue

        A.phase([R_tmp, R_m, R_y])
        mk = A.mark()
        w_u = A.alloc("w_u", [128, KC, 512], BF16)
        wpg = A.alloc("wpg", [128, 4, 128], BF16)
        pscol = A.alloc("pscol", [128, 4], F32)
        invc = A.alloc("invc", [128, 4, 16], F32)
        PADW = 16
        U2 = [A.alloc("U", [128, PADW + S], F32) for _ in range(2)]
        SA = A.alloc("SA", [128, PADW + S], F32)
        SB = A.alloc("SB", [128, PADW + S], F32)
        ypre2 = [A.alloc("ypre", [128, S], BF16) for _ in range(2)]
        tmp16 = A.alloc("tmp16", [128, 16], F32)
        dma("pool", w_u[:, :, :], w_in_d.rearrange(WKC, p=128)[:, :, 0:512], [], [("w_u",)], "w_u")
        dma("pool", wpg[:, :, :], wpg_d.rearrange("g c e -> c g e"), [], [("wpg",)], "wpg")
        dma("sp", pscol[:, :], pscale_d.rearrange("(g p) -> p g", p=128), [], [("pscol",)], "pscol",
            allow_slow_non_contiguous=True)
        dma("sp", invc[:, :, :], invc_d[:, :, :], [], [("invc",)], "invc")
        for buf, nm in ((U2[0], ("U", 0)), (U2[1], ("U", 1)), (SA, "SA"), (SB, "SB")):
            memset("pool", buf[:, 0:PADW], 0.0, [nm if isinstance(nm, tuple) else (nm,)])

        def pool_proj(g):
            U = U2[g % 2]
            for blk in range(NQB):
                bi = 2 + (blk % 2)
                for kc in range(KC):
                    mm(bank(bi), w_u[:, kc, g * 128:(g + 1) * 128], hT[:, kc, blk * 512:(blk + 1) * 512], kc == 0, kc == KC - 1,
                       [("w_u",), ("hT", kc, blk)], [PT(bi)])
                cp("act", U[:, PADW + blk * 512:PADW + (blk + 1) * 512], bank(bi), [PT(bi)], [("U", g % 2)])

        def pool_chain(g):
            w = 2 << g
            U, Un = U2[g % 2], ("U", g % 2)
            ypre, yn = ypre2[g % 2], ("ypre", g % 2)
            src, srcn = U, Un
            bufs = [(SA, ("SA",)), (SB, ("SB",))]
            sh, k = 1, 0
            while sh < w:
                dst, dstn = bufs[k % 2]
                tt("dve", dst[:, PADW:PADW + S], src[:, PADW:PADW + S], src[:, PADW - sh:PADW + S - sh], ALU.add,
                   [srcn], [dstn])
                src, srcn = dst, dstn
                sh *= 2
                k += 1
            stt("dve", ypre[:, :], src[:, PADW:PADW + S], 1.0 / w, U[:, PADW:PADW + S], ALU.mult, ALU.subtract,
                [srcn, Un], [yn])
            tt("dve", tmp16[:, :], src[:, PADW:PADW + 16], invc[:, g, :], ALU.mult, [srcn, ("invc",)], [("tmp16",)])
            tt("dve", ypre[:, 0:16], tmp16[:, :], U[:, PADW:PADW + 16], ALU.subtract, [("tmp16",), Un, yn], [yn])

        def pool_grp(g):
            ypre, yn = ypre2[g % 2], ("ypre", g % 2)
            for blk in range(NQB):
                bi = 4 + (blk % 2)
                mm(bank(bi), wpg[:, g, :], ypre[:, blk * 512:(blk + 1) * 512], True, True, [("wpg",), yn], [PT(bi)])
                act(ypg[:, g, blk * 512:(blk + 1) * 512], bank(bi), AF.Identity, [PT(bi), ("pscol",)], [("ypg", g, blk)],
                    scale=pscol[:, g:g + 1])

        pool_proj(0)
        for g in range(4):
            if g + 1 < 4:
                pool_proj(g + 1)
            pool_chain(g)
            pool_grp(g)
        A.release(mk)
        sc.barrier()
        ypg_all = [("ypg", g, blk) for g in range(4) for blk in range(NQB)]
        if stage_i == 1:
            if b == 0:
                dump("dbg_ypg", ypg[:, :, :], [128, 4, S], BF16, ypg_all)
            for t in range(NT):
                dma("sp", out_d[b, t * 128:(t + 1) * 128, :], acc[:, t, :], [("acc", t)], [("outdram", b, t)], "out")
            A.release(mk_b)
            sc.barrier()
            continue

        A.phase([R_tmp, R_m])
        mk = A.mark()
        w_qkv = A.alloc("w_qkv", [128, KC, 1536], BF16)
        for hp_ in range(4):
            for sec in (2, 0, 1):
                c0 = sec * 512 + hp_ * 128
                dma("pool", w_qkv[:, :, c0:c0 + 128], w_in_d.rearrange(WKC, p=128)[:, :, 512 + c0:512 + c0 + 128], [],
                    [("w_qkv", sec, hp_)], f"w_qkv{sec}_{hp_}")
        qa = [A.alloc("qa", [128, S], BF16) for _ in range(2)]
        ka = [A.alloc("ka", [128, S], BF16) for _ in range(2)]
        va2 = [A.alloc("va", [128, NT, 2, 128], BF16) for _ in range(2)]
        EMT = A.alloc("EMT", [128, H, 2, 128], BF16)
        b31 = A.alloc("b31", [128, H], F32)
        nb31 = A.alloc("nb31", [128, H], F32)
        maskd = A.alloc("maskd", [128, 128], F32)
        pastb = A.alloc("pastb", [128, NT, 8], F32)
        ownfx = A.alloc("ownfx", [128, NT, 8], F32)
        gm = A.alloc("gm", [128, NT, 8], F32)
        m8 = A.alloc("m8", [128, 8], F32)
        selt = A.alloc("selt", [128, NT, 8], F32)
        selp = A.alloc("selp", [128, NT, 72], BF16)
        kbar = A.alloc("kbar", [128, 8], F32)
        kbarb = A.alloc("kbarb", [128, 8], BF16)
        ebias2 = [A.alloc("ebias", [128, 128], F32) for _ in range(2)]
        ptb = [A.alloc("ptb", [128, 512], BF16) for _ in range(4)]
        sums = A.alloc("sums", [128, 512], F32)
        rsum = A.alloc("rsum", [128, 512], F32)
        dma("sp", b31[:, :], b31_d[:, :], [], [("b31",)], "attc1")
        dma("sp", maskd[:, :], maskd_d[:, :], [], [("maskd",)], "attc2")
        dma("sp", pastb[:, :, :], pastb_d[:, :, :], [], [("pastb",)], "attc3")
        dma("sp", ownfx[:, :, :], ownfix_d[:, :, :], [], [("ownfx",)], "attc4")
        for i in range(2):
            dma("sp", ka[i][64:72, :], ind_d[:, :], [], [("ka_ind", i)], f"attc5_{i}")
        ts("dve", nb31[:, :], b31[:, :], -1.0, None, ALU.mult, None, [("b31",)], [("nb31",)])
        for vb_ in va2:
            memset("pool", vb_[:, :, 0, 64:128], 1.0, [("va_ones",)])
            memset("pool", vb_[:, :, 1, 0:64], 1.0, [("va_ones",)])
        memset("pool", selp[:, :, :], 0.0, [("selp",)])
        def prepA(h):
            hp, hh = divmod(h, 2)
            vb = va2[hp % 2]
            if hh == 0:
                for t in range(NT):
                    bi = 6 + t % 2
                    for kc in range(KC):
                        mm(bank(bi)[:, 0:128], hT[:, kc, t * 128:(t + 1) * 128], w_qkv[:, kc, 1024 + hp * 128:1024 + (hp + 1) * 128],
                           kc == 0, kc == KC - 1, [("w_qkv", 2, hp), ("hT", kc, t // 4)], [PT(bi)])
                    cp("act", vb[:, t, 0, 0:64], bank(bi)[:, 0:64], [PT(bi), ("va_ones",)], [("va", hp % 2, t)])
                    cp("dve", vb[:, t, 1, 64:128], bank(bi)[:, 64:128], [PT(bi), ("va_ones",)], [("va", hp % 2, t)])
            for which, dst, dn in ((0, qa[hh], "qa"), (1, ka[hh], "ka")):
                for blk in range(NQB):
                    bi = 6 + (blk % 2)
                    c0 = which * 512 + h * 64
                    for kc in range(KC):
                        mm(bank(bi)[0:64, :], w_qkv[:, kc, c0:c0 + 64], hT[:, kc, blk * 512:(blk + 1) * 512],
                           kc == 0, kc == KC - 1, [("w_qkv", which, hp), ("hT", kc, blk)], [PT(bi)])
                    cp("act" if blk % 2 == 0 else "dve", dst[0:64, blk * 512:(blk + 1) * 512], bank(bi)[0:64, :],
                       [PT(bi)], [(dn, hh, blk)])
            ka_all = [("ka", hh, blk) for blk in range(NQB)]
            sc.add("dve", "tensor_reduce", dict(out=kbar[0:64, :], in_=ka[hh][0:64, :].rearrange("p (j l) -> p j l", l=256),
                                                axis=AX.X, op=ALU.add), ka_all, [("kbar",)])
            cp("dve", kbarb[0:64, :], kbar[0:64, :], [("kbar",)], [("kbarb",)])
            th = []

            def gate_mm():
                for t in range(NT):
                    mm(bank(6)[:, t * 8:(t + 1) * 8], qa[hh][0:64, t * 128:(t + 1) * 128], kbarb[0:64, :], True, True,
                       [("qa", hh, t // 4), ("kbarb",)], [PT(6)])
            th.append(gate_mm)
            th.append(lambda: tt("dve", gm[:, :, :], bank(6)[:, 0:128].rearrange("p (t j) -> p t j", j=8), pastb[:, :, :], ALU.add,
                                 [PT(6), ("pastb",)], [("gm",)]))
            for t in range(8, NT):
                def mx(t=t):
                    sc.add("dve", "max", dict(out=m8[:, :], in_=gm[:, t, :]), [("gm",)], [("m8",)])
                    ts("dve", selt[:, t, :], gm[:, t, :], m8[:, 2:3], None, ALU.is_ge, None, [("gm",), ("m8",)], [("selt",)])
                th.append(mx)

            def fin():
                ts("dve", selt[:, 8:, :], selt[:, 8:, :], -1.0, -NEG, ALU.add, ALU.mult, [("selt",)], [("selt",)])
                tt("dve", selp[:, 8:, 64:72], selt[:, 8:, :], ownfx[:, 8:, :], ALU.max, [("selt",), ("ownfx",), ("selp",)], [("selp",)])
            th.append(fin)
            return th

        def prepB(h):
            hh = h % 2
            pbs = bank(7).bitcast(BF16)
            for t in range(8, NT):
                tr(pbs[0:72, (t - 8) * 128:(t - 7) * 128], selp[:, t, :], [("selp",)], [PT(7)])
            memset("pool", qa[hh][64:72, 0:1024], 0.0, [("qa_sel", hh)])
            cp("act", qa[hh][64:72, 1024:2048], pbs[64:72, 0:1024], [PT(7), ("qa_sel", hh)], [("qa_sel", hh)])

        all_items = [(h, qb, kt) for h in range(H) for qb in range(NQB) for kt in range(4 * qb + 4)]
        NI = len(all_items)
        NPH = NI // H
        LA = 2

        def emit_norm(h, qb, ob):
            hp, hh = divmod(h, 2)
            q0 = qb * 512
            if hh == 0:
                mm(bank(3)[0:64, :], identf[64:128, 64:128], sums[64:128, :], True, True, [("identf",), ("sums",)], [PT(3)])
                sc.add("dve", "reciprocal", dict(out=rsum[0:64, :], in_=bank(3)[0:64, :]), [PT(3)], [("rsum",)])
                tt("dve", attnT[0:64, hp, q0:q0 + 512], bank(ob)[0:64, :], rsum[0:64, :], ALU.mult,
                   [PT(ob), ("rsum",)], [("attnT", hp, qb)])
            else:
                mm(bank(3)[:, :], shiftb[0:64, :], sums[0:64, :], True, True, [("shiftb",), ("sums",)], [PT(3)])
                sc.add("dve", "reciprocal", dict(out=rsum[64:128, :], in_=bank(3)[64:128, :]), [PT(3)], [("rsum",)])
                tt("dve", attnT[64:128, hp, q0:q0 + 512], bank(ob)[64:128, :], rsum[64:128, :], ALU.mult,
                   [PT(ob), ("rsum",)], [("attnT", hp, qb)])

        for f_ in prepA(0):
            f_()
        prepB(0)
        for h in range(H):
            for off in range(2):
                ek = (h * 2 + off) % 2
                eb = ebias2[ek]
                dma("sp", eb[:, :], biasT_d[h, off, :, :], [], [("ebias", ek)], f"ebias{ek}")
                act(eb[:, :], eb[:, :], AF.Exp, [("ebias", ek), ("nb31",)], [("ebias", ek)], bias=nb31[:, h:h + 1], scale=1.0)
                if off == 0:
                    tt("dve", EMT[:, h, off, :], eb[:, :], maskd[:, :], ALU.mult, [("ebias", ek), ("maskd",)], [("EMT", h)])
                else:
                    cp("dve", EMT[:, h, off, :], eb[:, :], [("ebias", ek)], [("EMT", h)])
        deferred = []
        pend = []
        for i in range(NI + LA):
            if i < NI:
                h, qb, kt = all_items[i]
                hp, hh = divmod(h, 2)
                if i % NPH == 0 and h + 1 < H:
                    deferred = prepA(h + 1) + [lambda h1=h + 1: prepB(h1)]
                c0 = max(0, kt - 4 * qb) * 128
                sb = i % 3
                ps_ = i % 4
                q0 = qb * 512
                mm(bank(sb)[:, c0:512], ka[hh][0:72, kt * 128:(kt + 1) * 128], qa[hh][0:72, q0 + c0:q0 + 512], True, True,
                   [("ka", hh, kt // 4), ("ka_ind", hh), ("qa", hh, qb), ("qa_sel", hh)], [PT(sb)])
                act(ptb[ps_][:, c0:512], bank(sb)[:, c0:512], AF.Exp, [PT(sb), ("b31",)], [("ptb", ps_)],
                    bias=b31[:, h:h + 1], scale=DH ** -0.5)
                for qi in range(4):
                    off = (4 * qb + qi) - kt
                    if off in (0, 1):
                        tt("dve", ptb[ps_][:, qi * 128:(qi + 1) * 128], ptb[ps_][:, qi * 128:(qi + 1) * 128],
                           EMT[:, h, off, :], ALU.mult, [("ptb", ps_), ("EMT", h)], [("ptb", ps_)])
                if i % NPH >= 3 and deferred:
                    deferred.pop(0)()
            j = i - LA
            if j >= 0:
                h2, qb, kt = all_items[j]
                hp2, hh2 = divmod(h2, 2)
                vb = va2[hp2 % 2]
                c0 = max(0, kt - 4 * qb) * 128
                ps_ = j % 4
                ob = 4 + (h2 * NQB + qb) % 2
                nkt = 4 * qb + 4
                mm(bank(ob)[:, c0:512], vb[:, kt, hh2, :], ptb[ps_][:, c0:512], kt == 0, kt == nkt - 1,
                   [("va", hp2 % 2, kt), ("va_ones",), ("ptb", ps_)], [PT(ob)])
                for ph, pq, pob in pend:
                    emit_norm(ph, pq, pob)
                pend = []
                if kt == nkt - 1:
                    if hh2 == 0:
                        cp("act", sums[64:128, :], bank(ob)[64:128, :], [PT(ob)], [("sums",)])
                    else:
                        cp("act", sums[0:64, :], bank(ob)[0:64, :], [PT(ob)], [("sums",)])
                    pend.append((h2, qb, ob))
        for ph, pq, pob in pend:
            emit_norm(ph, pq, pob)
        while deferred:
            deferred.pop(0)()
        A.release(mk)
        sc.barrier()
        attn_all = [("attnT", hp, qb) for hp in range(4) for qb in range(NQB)]
        if stage_i == 2:
            if b == 0:
                dump("dbg_attnT", attnT[:, :, :], [128, 4, S], BF16, attn_all)
            for t in range(NT):
                dma("sp", out_d[b, t * 128:(t + 1) * 128, :], acc[:, t, :], [("acc", t)], [("outdram", b, t)], "out")
            A.release(mk_b)
            sc.barrier()
            continue

        A.phase([R_tmp])
        mk = A.mark()
        bgcol = A.alloc("bgcol", [128, 16], F32)
        dma("sp", bgcol[:, :], bgate_d.rearrange("(c p) -> p c", p=128), [], [("bgcol",)], "bgcol", allow_slow_non_contiguous=True)
        wgl = [A.alloc("wgl", [128, KC, 2, 256], BF16) for _ in range(2)]
        wpu = [A.alloc("wpu", [128, 4, 256], BF16) for _ in range(2)]
        wau = [A.alloc("wau", [128, 4, 256], BF16) for _ in range(2)]
        G0 = [A.alloc("G0", [128, 512], F32) for _ in range(2)]
        G1 = [A.alloc("G1", [128, 512], F32) for _ in range(2)]
        M1 = [A.alloc("M1", [128, 512], F32) for _ in range(2)]
        M2 = [A.alloc("M2", [128, 512], F32) for _ in range(2)]

        def a3_load(qt):
            ws = qt % 2
            for gi in range(2):
                c0 = 2048 + gi * 1024 + qt * 256
                dma("pool", wgl[ws][:, :, gi, :], w_in_d.rearrange(WKC, p=128)[:, :, c0:c0 + 256], [], [("wgl", ws)], f"wgl{ws}")
            dma("pool", wpu[ws][:, :, :], wpu_d.rearrange("(g p) n -> p g n", p=128)[:, :, qt * 256:(qt + 1) * 256], [], [("wpu", ws)], f"wpu{ws}")
            dma("pool", wau[ws][:, :, :], wau_d.rearrange("(g p) n -> p g n", p=128)[:, :, qt * 256:(qt + 1) * 256], [], [("wau", ws)], f"wau{ws}")

        it = 0
        a3_load(0)
        a3_load(1)
        for qt in range(4):
            ws = qt % 2
            for dcl in range(2):
                dc = qt * 2 + dcl
                for blk in range(NQB):
                    s2 = it % 2
                    pb0 = 4 * s2
                    cs = slice(blk * 512, (blk + 1) * 512)
                    for gi in range(2):
                        for kc in range(KC):
                            mm(bank(pb0 + gi), wgl[ws][:, kc, gi, dcl * 128:(dcl + 1) * 128], hT[:, kc, cs], kc == 0, kc == KC - 1,
                               [("wgl", ws), ("hT", kc, blk)], [PT(pb0 + gi)])
                        act((G0 if gi == 0 else G1)[s2][:, :], bank(pb0 + gi), AF.Sigmoid, [PT(pb0 + gi), ("bgcol",)],
                            [("G", gi, s2)], bias=bgcol[:, gi * 8 + dc:gi * 8 + dc + 1], scale=1.0)
                    for g in range(4):
                        mm(bank(pb0 + 2), wpu[ws][:, g, dcl * 128:(dcl + 1) * 128], ypg[:, g, cs], g == 0, g == 3,
                           [("wpu", ws), ("ypg", g, blk)], [PT(pb0 + 2)])
                    for hp in range(4):
                        mm(bank(pb0 + 3), wau[ws][:, hp, dcl * 128:(dcl + 1) * 128], attnT[:, hp, cs], hp == 0, hp == 3,
                           [("wau", ws), ("attnT", hp, blk)], [PT(pb0 + 3)])
                    tt("dve", M1[s2][:, :], G0[s2][:, :], bank(pb0 + 2), ALU.mult, [("G", 0, s2), PT(pb0 + 2)], [("M1", s2)])
                    tt("dve", M2[s2][:, :], G1[s2][:, :], bank(pb0 + 3), ALU.mult, [("G", 1, s2), PT(pb0 + 3)], [("M2", s2)])
                    tt("pool", mT[:, dc, cs], M1[s2][:, :], M2[s2][:, :], ALU.add, [("M1", s2), ("M2", s2)], [("mT", dc, blk)])
                    it += 1
            if qt + 2 < 4:
                a3_load(qt + 2)
        A.release(mk)
        sc.barrier()
        mT_all = [("mT", dc, blk) for dc in range(KC) for blk in range(NQB)]
        if stage_i == 3:
            if b == 0:
                dump("dbg_mT", mT[:, :, :], [128, KC, S], BF16, mT_all)
            for t in range(NT):
                dma("sp", out_d[b, t * 128:(t + 1) * 128, :], acc[:, t, :], [("acc", t)], [("outdram", b, t)], "out")
            A.release(mk_b)
            sc.barrier()
            continue

        A.phase([R_tmp, R_x, R_y])
        mk = A.mark()
        lnbc = A.alloc("lnbc", [128, 2, D], F32)
        load_lnbc(lnbc, 2)
        wout = A.alloc("wout", [128, KC, D], BF16)
        for hf_ in range(2):
            dma("pool", wout[:, :, hf_ * 512:(hf_ + 1) * 512], wout_d.rearrange(WKC, p=128)[:, :, hf_ * 512:(hf_ + 1) * 512], [],
                [("wout", hf_)], f"wout{hf_}")
        r1 = [A.alloc("r1", [128, D], F32) for _ in range(4)]
        xn = [A.alloc("xn", [128, D], BF16) for _ in range(4)]
        st = [A.alloc("st", [128, 2, 6], F32) for _ in range(4)]
        mv = [A.alloc("mv", [128, 4], F32) for _ in range(4)]
        def b1_mm(t):
            blk = t // 4
            pt_ = 1 + t % 2
            for hf in range(2):
                for dc in range(KC):
                    mm(P[pt_][:, hf * 512:(hf + 1) * 512], mT[:, dc, t * 128:(t + 1) * 128], wout[:, dc, hf * 512:(hf + 1) * 512],
                       dc == 0, dc == KC - 1, [("mT", dc, blk), ("wout", hf)], [PT(2 * pt_ + hf)])

        def b1_post(t):
            s4 = t % 4
            pt_ = 1 + t % 2
            stt("dve", r1[s4][:, :], acc[:, t, :], ALPHA, P[pt_][:, :], ALU.mult, ALU.add,
                [("acc", t), PT(2 * pt_), PT(2 * pt_ + 1)], [("r1", s4)])
            layer_norm_stats(r1[s4], ("r1", s4), st[s4], mv[s4], s4)
            act(xn[s4][:, :], r1[s4][:, :], AF.Identity, [("r1", s4), ("mv", s4)], [("xn", s4)],
                bias=mv[s4][:, 3:4], scale=mv[s4][:, 2:3])
            act(acc[:, t, :], r1[s4][:, :], AF.Identity, [("r1", s4), ("mv", s4)], [("acc", t)],
                bias=mv[s4][:, 3:4], scale=mv[s4][:, 2:3])
            tt("pool", acc[:, t, :], acc[:, t, :], lnbc[:, 0, :], ALU.mult, [("acc", t), ("lnbc", 2)], [("acc", t)])
            tt("pool", acc[:, t, :], acc[:, t, :], lnbc[:, 1, :], ALU.add, [("acc", t), ("lnbc", 2)], [("acc", t)])

        done_mm = set()
        for blk in range(NQB):
            for ti in range(4):
                t = blk * 4 + ti
                if t not in done_mm:
                    b1_mm(t)
                b1_post(t)
            for t2 in (blk * 4 + 4, blk * 4 + 5):
                if t2 < NT:
                    b1_mm(t2)
                    done_mm.add(t2)
            transposes_to_T(xn, hT, blk, 2, 3, "xn", "hT")
        A.release(mk)
        sc.barrier()
        if stage_i == 4:
            for t in range(NT):
                dma("sp", out_d[b, t * 128:(t + 1) * 128, :], acc[:, t, :], [("acc", t)], [("outdram", b, t)], "out")
            A.release(mk_b)
            sc.barrier()
            continue

        A.phase([R_tmp, R_x, R_y])
        mk = A.mark()
        lnbc = A.alloc("lnbc", [128, 2, D], F32)
        load_lnbc(lnbc, 4)
        wq = A.alloc("wq", [128, KC, 512], BF16)
        A2 = SegArena(nc, [R_m])
        wk = A2.alloc("wk", [128, KC, 512], BF16)
        wv = A2.alloc("wv", [128, KC, 512], BF16)
        wo = A.alloc("wo", [128, MH, D], BF16)
        memb = A2.alloc("memb", [128, 2, D], BF16)
        memT = A2.alloc("memT", [128, KC, MEMLEN], BF16)
        kmT = A.alloc("kmT", [128, MH, MEMLEN], BF16)
        vm = A.alloc("vm", [128, 2, 512], BF16)
        dma("pool", memb[:, :, :], mem_d[b].rearrange("(t p) d -> p t d", p=128), [], [("memb",)], "memb")
        dma("pool", wk[:, :, :], wmk_d.rearrange(WKC, p=128), [], [("wk",)], "wk")
        dma("pool", wv[:, :, :], wmv_d.rearrange(WKC, p=128), [], [("wv",)], "wv")
        dma("pool", wq[:, :, :], wmq_d.rearrange(WKC, p=128), [], [("wq",)], "wq")
        dma("pool", wo[:, :, :], wmo_d.rearrange(WKC, p=128), [], [("wo",)], "wo")
        for kc in range(KC):
            bi = kc % 2
            pb = bank(bi).bitcast(BF16)
            for j in range(2):
                tr(pb[:, j * 128:(j + 1) * 128], memb[:, j, kc * 128:(kc + 1) * 128], [("memb",)], [PT(bi)])
            cp("act" if kc % 2 == 0 else "dve", memT[:, kc, :], pb[:, 0:256], [PT(bi)], [("memT", kc)])
        memT_all = [("memT", kc) for kc in range(KC)]
        for hm in range(MH):
            bi = 2 + hm % 2
            for kc in range(KC):
                mm(bank(bi)[:, 0:256], wk[:, kc, hm * 128:(hm + 1) * 128], memT[:, kc, :], kc == 0, kc == KC - 1,
                   [("wk",), ("memT", kc)], [PT(bi)])
            cp("act", kmT[:, hm, :], bank(bi)[:, 0:256], [PT(bi)], [("kmT",)])
        for j in range(2):
            bi = 4 + j
            for kc in range(KC):
                mm(bank(bi), memT[:, kc, j * 128:(j + 1) * 128], wv[:, kc, :], kc == 0, kc == KC - 1, [("wv",), ("memT", kc)], [PT(bi)])
            cp("dve", vm[:, j, :], bank(bi), [PT(bi)], [("vm",)])
        sc.barrier()
        q1T = A.alloc("q1T", [128, MH, 512], BF16)
        oT = A.alloc("oT", [128, MH, 512], BF16)
        pm = [A.alloc("pm", [128, 512], BF16) for _ in range(4)]
        rs2 = [A.alloc("rs2", [128, 512], F32) for _ in range(2)]
        r1 = [A.alloc("r2", [128, D], F32) for _ in range(4)]
        xn = [A.alloc("xn", [128, D], BF16) for _ in range(4)]
        st = [A.alloc("st", [128, 2, 6], F32) for _ in range(4)]
        mv = [A.alloc("mv", [128, 4], F32) for _ in range(4)]

        def b2_qproj(qb):
            cs = slice(qb * 512, (qb + 1) * 512)
            for hm in range(MH):
                bi = hm % 2
                for kc in range(KC):
                    mm(bank(bi), wq[:, kc, hm * 128:(hm + 1) * 128], hT[:, kc, cs], kc == 0, kc == KC - 1,
                       [("wq",), ("hT", kc, qb)], [PT(bi)])
                cp("act" if hm % 2 == 0 else "dve", q1T[:, hm, :], bank(bi), [PT(bi)], [("q1T", hm)])

        def b2_attn(qb):
            items = [(hm, j) for hm in range(MH) for j in range(2)]
            LA = 2
            for i in range(len(items) + LA):
                if i < len(items):
                    hm, j = items[i]
                    sb = i % 4
                    mm(bank(sb), kmT[:, hm, j * 128:(j + 1) * 128], q1T[:, hm, :], True, True, [("kmT",), ("q1T", hm)], [PT(sb)])
                    act(pm[sb][:, :], bank(sb), AF.Exp, [PT(sb)], [("pm", sb)], scale=128.0 ** -0.5)
                k = i - LA
                if k >= 0:
                    hm, j = items[k]
                    sb = k % 4
                    ob = 4 + 2 * (hm % 2)
                    mm(bank(ob), vm[:, j, hm * 128:(hm + 1) * 128], pm[sb][:, :], j == 0, j == 1, [("vm",), ("pm", sb)], [PT(ob)])
                    mm(bank(ob + 1), onesb[:, :], pm[sb][:, :], j == 0, j == 1, [("onesb",), ("pm", sb)], [PT(ob + 1)])
                    if j == 1:
                        sc.add("dve", "reciprocal", dict(out=rs2[hm % 2][:, :], in_=bank(ob + 1)), [PT(ob + 1)], [("rs2", hm % 2)])
                        tt("dve", oT[:, hm, :], bank(ob), rs2[hm % 2][:, :], ALU.mult, [PT(ob), ("rs2", hm % 2)], [("oT", hm)])

        def b2_out_ln(qb):
            for ti in range(4):
                t = qb * 4 + ti
                s4 = ti
                po = 3 if ti % 2 == 0 else 0
                for hf in range(2):
                    for hm in range(MH):
                        mm(P[po][:, hf * 512:(hf + 1) * 512], oT[:, hm, ti * 128:(ti + 1) * 128], wo[:, hm, hf * 512:(hf + 1) * 512],
                           hm == 0, hm == MH - 1, [("oT", hm), ("wo",)], [PT(2 * po + hf)])
                stt("dve", r1[s4][:, :], acc[:, t, :], ALPHA, P[po][:, :], ALU.mult, ALU.add,
                    [("acc", t), PT(2 * po), PT(2 * po + 1)], [("r1", s4)])
                layer_norm_stats(r1[s4], ("r1", s4), st[s4], mv[s4], s4)
                act(xn[s4][:, :], r1[s4][:, :], AF.Identity, [("r1", s4), ("mv", s4)], [("xn", s4)],
                    bias=mv[s4][:, 3:4], scale=mv[s4][:, 2:3])
                act(acc[:, t, :], r1[s4][:, :], AF.Identity, [("r1", s4), ("mv", s4)], [("acc", t)],
                    bias=mv[s4][:, 3:4], scale=mv[s4][:, 2:3])
                tt("pool", acc[:, t, :], acc[:, t, :], lnbc[:, 0, :], ALU.mult, [("acc", t), ("lnbc", 4)], [("acc", t)])
                tt("pool", acc[:, t, :], acc[:, t, :], lnbc[:, 1, :], ALU.add, [("acc", t), ("lnbc", 4)], [("acc", t)])

        b2_qproj(0)
        for qb in range(NQB):
            b2_attn(qb)
            b2_out_ln(qb)
            if qb + 1 < NQB:
                b2_qproj(qb + 1)
            transposes_to_T(xn, mT, qb, 4, 5, "xn", "mT")
        A.release(mk)
        sc.barrier()
        if stage_i == 5:
            for t in range(NT):
                dma("sp", out_d[b, t * 128:(t + 1) * 128, :], acc[:, t, :], [("acc", t)], [("outdram", b, t)], "out")
            A.release(mk_b)
            sc.barrier()
            continue

        A.phase([R_tmp, R_x, R_y, R_hT])
        mk = A.mark()
        lnbc = A.alloc("lnbc", [128, 2, D], F32)
        load_lnbc(lnbc, 6)
        wr = A.alloc("wr", [128, KC, 36], BF16)
        brb = A.alloc("brb", [128, 36], F32)
        dma("pool", wr[:, :, :], wr_d.rearrange(WKC, p=128), [], [("wr",)], "wr")
        dma("sp", brb[:, :], br_d.partition_broadcast(128), [], [("brb",)], "brb")
        comb = A.alloc("comb", [128, NT, NE], F32)
        lg = A.alloc("lg", [128, 36], F32)
        cm = A.alloc("cm", [128, 8], F32)
        ec = A.alloc("ec", [128, 4], F32)
        oh = A.alloc("oh", [128, 4], F32)
        ef = A.alloc("ef", [128, 4, 8], F32)
        fm = A.alloc("fm", [128, 4], F32)
        v8 = A.alloc("v8", [128, 4, 8], F32)
        sg = A.alloc("sg", [128, 4], F32)
        msk = A.alloc("msk", [128, 4, 8], F32)
        def router(t):
            for kc in range(KC):
                mm(bank(5)[:, 0:36], mT[:, kc, t * 128:(t + 1) * 128], wr[:, kc, :], kc == 0, kc == KC - 1,
                   [("mT", kc, t // 4), ("wr",)], [PT(5)])
            tt("dve", lg[:, :], bank(5)[:, 0:36], brb[:, :], ALU.add, [PT(5), ("brb",)], [("lg",)])
            sc.add("dve", "tensor_reduce", dict(out=cm[:, 0:1], in_=lg[:, 0:4], axis=AX.X, op=ALU.max), [("lg",)], [("cm",)])
            ts("dve", oh[:, :], lg[:, 0:4], cm[:, 0:1], None, ALU.is_ge, None, [("lg",), ("cm",)], [("oh",)])
            ts("dve", cm[:, 1:2], cm[:, 0:1], -1.0, None, ALU.mult, None, [("cm",)], [("cm",)])
            act(ec[:, :], lg[:, 0:4], AF.Exp, [("lg",), ("cm",)], [("ec",)], bias=cm[:, 1:2], scale=1.0)
            sc.add("dve", "tensor_reduce", dict(out=cm[:, 2:3], in_=ec[:, :], axis=AX.X, op=ALU.add), [("ec",)], [("cm",)])
            sc.add("dve", "reciprocal", dict(out=cm[:, 3:4], in_=cm[:, 2:3]), [("cm",)], [("cm",)])
            lf = lg[:, 4:36].rearrange("p (g j) -> p g j", j=8)
            sc.add("dve", "tensor_reduce", dict(out=fm[:, :], in_=lf, axis=AX.X, op=ALU.max), [("lg",)], [("fm",)])
            tt("dve", ef[:, :, :], lf, fm[:, :].unsqueeze(2).to_broadcast([128, 4, 8]), ALU.subtract, [("lg",), ("fm",)], [("ef",)])
            act(ef[:, :, :], ef[:, :, :], AF.Exp, [("ef",)], [("ef",)])
            for g in range(4):
                sc.add("dve", "max", dict(out=v8[:, g, :], in_=ef[:, g, :]), [("ef",)], [("v8",)])
            ts("dve", sg[:, :], v8[:, :, 1], 1.0, None, ALU.add, None, [("v8",)], [("sg",)])
            sc.add("dve", "reciprocal", dict(out=sg[:, :], in_=sg[:, :]), [("sg",)], [("sg",)])
            stt("dve", sg[:, :], sg[:, :], cm[:, 3:4], oh[:, :], ALU.mult, ALU.mult, [("sg",), ("cm",), ("oh",)], [("sg",)])
            tt("dve", msk[:, :, :], ef[:, :, :], v8[:, :, 1:2].to_broadcast([128, 4, 8]), ALU.is_ge, [("ef",), ("v8",)], [("msk",)])
            tt("dve", msk[:, :, :], msk[:, :, :], ef[:, :, :], ALU.mult, [("msk",), ("ef",)], [("msk",)])
            tt("dve", comb[:, t, :].rearrange("p (g j) -> p g j", j=8), msk[:, :, :],
               sg[:, :].unsqueeze(2).to_broadcast([128, 4, 8]), ALU.mult, [("msk",), ("sg",)], [("comb", t)])
        wgu = [A.alloc("wgu", [128, 2, KC, 512], BF16) for _ in range(2)]
        wdn = [A.alloc("wdn", [128, 2, 2, D], BF16) for _ in range(2)]
        sa = [[A.alloc("sa", [128, 256], F32) for _ in range(2)] for _ in range(2)]
        hid = [[A.alloc("hid", [128, 256], BF16) for _ in range(2)] for _ in range(2)]
        hidT = [A.alloc("hidT", [128, 2, 2, 128], BF16) for _ in range(2)]
        NP_ = NE // 2
        NU = NP_ * NT
        pb4 = P[2][:, 0:512].bitcast(BF16)

        def w_dma(pr):
            ws = pr % 2
            for ei in range(2):
                e = pr * 2 + ei
                dma("pool", wgu[ws][:, ei, :, 0:256], wg_d[e].rearrange(WKC, p=128), [], [("wgu", ws)], f"wgu{ws}")
                dma("pool", wgu[ws][:, ei, :, 256:512], wu_d[e].rearrange(WKC, p=128), [], [("wgu", ws)], f"wgu{ws}")
            for ei in range(2):
                e = pr * 2 + ei
                dma("pool", wdn[ws][:, ei, :, :], wd_d[e].rearrange("(c p) d -> p c d", p=128), [], [("wdn", ws)], f"wdn{ws}")

        def u_up(u):
            pr, t = divmod(u, NT)
            ws, par = pr % 2, u % 2
            for kc in range(KC):
                for ei in range(2):
                    mm(bank(2 * par + ei), mT[:, kc, t * 128:(t + 1) * 128], wgu[ws][:, ei, kc, :], kc == 0, kc == KC - 1,
                       [("mT", kc, t // 4), ("wgu", ws)], [PT(2 * par + ei)])

        def u_elem(u):
            pr, t = divmod(u, NT)
            par = u % 2
            for ei in range(2):
                e = pr * 2 + ei
                bk = 2 * par + ei
                act(sa[par][ei][:, :], bank(bk)[:, 0:256], AF.Silu, [PT(bk)], [("sa", par, ei)])
                stt("dve", hid[par][ei][:, :], sa[par][ei][:, :], comb[:, t, e:e + 1], bank(bk)[:, 256:512], ALU.mult, ALU.mult,
                    [("sa", par, ei), ("comb", t), PT(bk)], [("hid", par, ei)])

        def u_tr(u):
            par = u % 2
            for ei in range(2):
                for c in range(2):
                    col = par * 512 + (ei * 2 + c) * 128
                    tr(pb4[:, col:col + 128], hid[par][ei][:, c * 128:(c + 1) * 128], [("hid", par, ei)], [("ps4", par)])
            cp("act", hidT[par][:, :, :, :].rearrange("p e c t -> p (e c t)"), pb4[:, par * 512:(par + 1) * 512],
               [("ps4", par)], [("hidT", par)])

        def u_down(u):
            pr, t = divmod(u, NT)
            ws, par = pr % 2, u % 2
            for hf in range(2):
                k = 0
                for ei in range(2):
                    for c in range(2):
                        mm(P[3][:, hf * 512:(hf + 1) * 512], hidT[par][:, ei, c, :], wdn[ws][:, ei, c, hf * 512:(hf + 1) * 512],
                           k == 0, k == 3, [("hidT", par), ("wdn", ws)], [PT(6 + hf)])
                        k += 1
            if pr == 0:
                stt("dve", acc[:, t, :], acc[:, t, :], ALPHA, P[3][:, :], ALU.mult, ALU.add,
                    [("acc", t), PT(6), PT(7)], [("acc", t)])
            else:
                tt("dve", acc[:, t, :], acc[:, t, :], P[3][:, :], ALU.add, [("acc", t), PT(6), PT(7)], [("acc", t)])

        st3 = [A.alloc("st", [128, 2, 6], F32) for _ in range(2)]
        mv3 = [A.alloc("mv", [128, 4], F32) for _ in range(2)]
        ob_ = [A.alloc("ob", [128, D], F32) for _ in range(2)]

        def ln3_store(t):
            s2 = t % 2
            layer_norm_stats(acc[:, t, :], ("acc", t), st3[s2], mv3[s2], ("ln3", s2))
            act(ob_[s2][:, :], acc[:, t, :], AF.Identity, [("acc", t), ("mv", ("ln3", s2))], [("ob", s2)],
                bias=mv3[s2][:, 3:4], scale=mv3[s2][:, 2:3])
            tt("pool", ob_[s2][:, :], ob_[s2][:, :], lnbc[:, 0, :], ALU.mult, [("ob", s2), ("lnbc", 6)], [("ob", s2)])
            tt("pool", ob_[s2][:, :], ob_[s2][:, :], lnbc[:, 1, :], ALU.add, [("ob", s2), ("lnbc", 6)], [("ob", s2)])
            dma("sp", out_d[b, t * 128:(t + 1) * 128, :], ob_[s2][:, :], [("ob", s2)], [("outdram", b, t), ("ob", s2)], f"out{s2}")

        w_dma(0)
        w_dma(1)
        for t_ in range(3):
            router(t_)
        u_up(0)
        u_elem(0)
        for u in range(NU):
            pr, t = divmod(u, NT)
            if t == 0 and pr >= 1 and pr + 1 < NP_:
                w_dma(pr + 1)
            u_tr(u)
            if u + 1 < NU:
                u_up(u + 1)
                u_elem(u + 1)
            u_down(u)
            if pr == 0 and t + 3 < NT:
                router(t + 3)
            if pr == NP_ - 1:
                ln3_store(t)
        A.release(mk)
        A.release(mk_b)
        sc.barrier()

    fin = [("outdram", b, t) for b in range(nb) for t in range(NT)] + [("dbg", n) for n in dbg_out]
    sc.add("sp", "nop", dict(), fin, [])

    sc.finalize()
    keys = set()
    for op in sc.ops:
        for k, v in op.waits:
            keys.add(k)
        if op.dma_key is not None:
            keys.add(("dma", op.dma_key))
        elif op.sig:
            keys.add(("eng", op.eng))
    keys = sorted(keys)
    from contextlib import ExitStack
    with ExitStack() as es:
        sems = {k: es.enter_context(nc.semaphore(f"s_{k[0]}_{k[1]}")) for k in keys}
        block = es.enter_context(nc.Block())

        @block.tensor
        def _(e):
            sc.emit(nc, "pe", e, sems)

        @block.scalar
        def _(e):
            sc.emit(nc, "act", e, sems)

        @block.vector
        def _(e):
            sc.emit(nc, "dve", e, sems)

        @block.gpsimd
        def _(e):
            sc.emit(nc, "pool", e, sems)

        @block.sync
        def _(e):
            sc.emit(nc, "sp", e, sems)
    return nc, dbg_out, dict(n_ops=len(sc.ops), sbuf_peak=0, n_sems=len(keys))


def _rel_bucket_np(dist):
    d = np.maximum(dist, 0)
    large = 16 + (np.log(np.maximum(d, 1).astype(np.float32) / np.float32(16)) / np.float32(math.log(128 / 16))
                  * np.float32(16)).astype(np.int32)
    large = np.minimum(large, 31)
    return np.where(d < 16, d, large)


def make_inputs(inputs, nb=NB):
    f = lambda k: np.ascontiguousarray(np.asarray(inputs[k], dtype=np.float32))
    x = f("x")
    mem = f("mem")
    lnp = np.stack([f("ln_in_g").reshape(-1), f("ln_in_b").reshape(-1), f("ln1_g").reshape(-1), f("ln1_b").reshape(-1),
                    f("ln2_g").reshape(-1), f("ln2_b").reshape(-1), f("ln3_g").reshape(-1), f("ln3_b").reshape(-1)])
    rel = f("rel_bias")
    kk = np.arange(128)[:, None]
    qq = np.arange(128)[None, :]
    biasT = np.empty((H, 2, 128, 128), np.float32)
    for off in range(2):
        bk = _rel_bucket_np(off * 128 + qq - kk)
        biasT[:, off] = rel[bk].transpose(2, 0, 1)
    b31 = np.ascontiguousarray(np.broadcast_to(rel[31][None, :], (128, H)))
    maskd = (qq >= kk).astype(np.float32)
    tq = np.arange(NT)[:, None] // 2
    jj = np.arange(8)[None, :]
    pastbias = np.where(jj < tq, 0.0, -1e30).astype(np.float32)
    ownfix = np.where(jj == tq, 0.0, -1e9).astype(np.float32)
    pastbias = np.ascontiguousarray(np.broadcast_to(pastbias[None], (128, NT, 8)))
    ownfix = np.ascontiguousarray(np.broadcast_to(ownfix[None], (128, NT, 8)))
    ind = (np.arange(S)[None, :] // 256 == np.arange(8)[:, None]).astype(np.float32).astype(ml_dtypes.bfloat16)
    tt_ = np.arange(16)[None, :]
    ww = np.array([2, 4, 8, 16])[:, None]
    invc = (1.0 / np.minimum(tt_ + 1, ww)).astype(np.float32)
    invc = np.ascontiguousarray(np.broadcast_to(invc[None], (128, 4, 16)))
    shiftb = np.zeros((64, 128), np.float32)
    shiftb[np.arange(64), 64 + np.arange(64)] = 1.0
    common = {
        "lnp": lnp, "ident": np.eye(128, dtype=np.float32).astype(ml_dtypes.bfloat16), "identf": np.eye(128, dtype=np.float32),
        "shiftb": shiftb, "w_in": f("w_in")[0], "b_gate": f("b_gate")[0], "w_pool_grp": f("w_pool_grp")[0],
        "pool_scale": f("pool_scale")[0], "w_pool_up": f("w_pool_up")[0], "w_attn_up": f("w_attn_up")[0],
        "w_mix_out": f("w_mix_out")[0], "w_mq": f("w_mq")[0], "w_mk": f("w_mk")[0], "w_mv": f("w_mv")[0], "w_mo": f("w_mo")[0],
        "w_router": np.ascontiguousarray(np.concatenate([f("w_coarse")[0], f("w_fine")[0].reshape(D, 32)], axis=1)),
        "b_router": np.ascontiguousarray(np.concatenate([f("b_coarse")[0], f("b_fine")[0].reshape(32)])),
        "w_gate": f("w_gate")[0], "w_up": f("w_up")[0], "w_down": f("w_down")[0],
        "biasT": biasT, "b31": b31, "maskd": maskd, "pastbias": pastbias, "ownfix": ownfix, "indrows": ind, "invc": invc,
    }
    maps = []
    for c in range(NCORES):
        m = dict(common)
        m["x"] = x[c * nb:(c + 1) * nb]
        m["mem"] = mem[c * nb:(c + 1) * nb]
        maps.append(m)
    return maps


_CACHE = {}


def kernel(**inputs):
    if "nc" not in _CACHE:
        _CACHE["nc"] = build_program("all", debug=False)
    nc, _, _ = _CACHE["nc"]
    maps = make_inputs(inputs)
    res = run_bass_kernel_spmd(nc, maps, core_ids=list(range(NCORES)))
    outs = [np.asarray(r["out"]) for r in res.results]
    return np.concatenate(outs, axis=0).astype(np.float32)
```

```python
import math
import numpy as np
import ml_dtypes
import concourse.bass as bass
import concourse.mybir as mybir
from concourse.bass_utils import run_bass_kernel_spmd

F32 = mybir.dt.float32
BF16 = mybir.dt.bfloat16
AF = mybir.ActivationFunctionType
ALU = mybir.AluOpType
AX = mybir.AxisListType

D = 1024
S = 2048
NB = 2
NCORES = 8
KC = D // 128
NT = S // 128
NQB = S // 512
H = 8
DH = 64
MEMLEN = 256
MH = 4
NE = 32
FF = 256
ALPHA = 2.0 ** 0.25
EPS = 1e-5
NEG = -30000.0


class _Op:
    __slots__ = ("eng", "fn", "deps", "idx", "sig", "sigval", "dma_key", "dma_val", "waits")


class Sched:
    def __init__(self, same_engine_sync=True):
        self.ops = []
        self.lastw = {}
        self.readers = {}
        self.dma_count = {}
        self.same_engine_sync = same_engine_sync

    def add(self, eng, meth, kw, reads=(), writes=(), dma_key=None):
        op = _Op()
        op.eng, op.fn, op.idx, op.dma_key = eng, (meth, kw), len(self.ops), dma_key
        op.sig, op.sigval, op.dma_val, op.waits = False, 0, 0, []
        deps = set()
        for t in reads:
            w = self.lastw.get(t)
            if w is not None:
                deps.add(w)
        for t in writes:
            w = self.lastw.get(t)
            if w is not None:
                deps.add(w)
            for r in self.readers.get(t, ()):
                deps.add(r)
        deps.discard(op.idx)
        op.deps = deps
        for t in reads:
            self.readers.setdefault(t, []).append(op.idx)
        for t in writes:
            self.lastw[t] = op.idx
            self.readers[t] = []
        if dma_key is not None:
            self.dma_count[dma_key] = self.dma_count.get(dma_key, 0) + 1
            op.dma_val = 16 * self.dma_count[dma_key]
        self.ops.append(op)
        return op.idx

    def barrier(self, engines=("pe", "act", "dve", "pool", "sp")):
        last = {}
        for op in self.ops:
            if op.dma_key is not None:
                last[("dma", op.dma_key)] = op.idx
            else:
                last[("eng", op.eng)] = op.idx
        deps = set(last.values())
        for e in engines:
            op = _Op()
            op.eng, op.fn, op.idx, op.dma_key = e, ("nop", {}), len(self.ops), None
            op.sig, op.sigval, op.dma_val, op.waits = False, 0, 0, []
            op.deps = set(deps)
            self.ops.append(op)

    def finalize(self):
        ops = self.ops
        for op in ops:
            best = {}
            for d in op.deps:
                p = ops[d]
                if p.dma_key is not None:
                    continue
                if p.eng == op.eng and (p.eng == "pe" or not self.same_engine_sync) and op.dma_key is None:
                    continue
                if d > best.get(p.eng, -1):
                    best[p.eng] = d
            for d in best.values():
                ops[d].sig = True
        cnt = {}
        for op in ops:
            if op.sig:
                cnt[op.eng] = cnt.get(op.eng, 0) + 1
                op.sigval = cnt[op.eng]
        waited = {}
        for op in ops:
            need = {}
            for d in op.deps:
                p = ops[d]
                if p.dma_key is not None:
                    k, v = ("dma", p.dma_key), p.dma_val
                else:
                    if not p.sig:
                        continue
                    if p.eng == op.eng and (p.eng == "pe" or not self.same_engine_sync) and op.dma_key is None:
                        continue
                    k, v = ("eng", p.eng), p.sigval
                if v > need.get(k, 0):
                    need[k] = v
            w = waited.setdefault(op.eng, {})
            for k, v in need.items():
                if v > w.get(k, 0):
                    w[k] = v
                    op.waits.append((k, v))

    def emit(self, nc, eng_name, eng, sems):
        n = 0
        for op in self.ops:
            if op.eng != eng_name:
                continue
            for k, v in op.waits:
                eng.wait_ge(sems[k], v)
            ins = getattr(eng, op.fn[0])(**op.fn[1])
            if op.dma_key is not None:
                ins.then_inc(sems[("dma", op.dma_key)], 16)
            elif op.sig:
                ins.then_inc(sems[("eng", op.eng)], 1)
            n += 1
        return n


class Arena:
    def __init__(self, nc, start=16640, limit=224 * 1024):
        self.nc, self.off, self.limit, self.n = nc, start, limit, 0
        self.peak = 0

    def alloc(self, name, shape, dtype):
        nbytes = int(np.prod(shape[1:])) * (4 if dtype == F32 else 2)
        nbytes = (nbytes + 63) // 64 * 64
        self.n += 1
        t = self.nc.alloc_sbuf_tensor_at(f"{name}_{self.n}", list(shape), dtype, offset=self.off)
        self.off += nbytes
        self.peak = max(self.peak, self.off)
        assert self.off <= self.limit, f"SBUF arena overflow at {name}: {self.off}"
        return t

    def mark(self):
        return self.off

    def release(self, m):
        self.off = m


class SegArena:
    _n = [0]

    def __init__(self, nc, segs):
        self.nc = nc
        self.segs = [[a, a, a + sz] for a, sz in segs]

    def alloc(self, name, shape, dtype):
        nbytes = int(np.prod(shape[1:])) * (4 if dtype == F32 else 2)
        nbytes = (nbytes + 63) // 64 * 64
        for sg in self.segs:
            if sg[1] + nbytes <= sg[2]:
                SegArena._n[0] += 1
                t = self.nc.alloc_sbuf_tensor_at(f"{name}_s{SegArena._n[0]}", list(shape), dtype, offset=sg[1])
                sg[1] += nbytes
                return t
        raise AssertionError(f"SegArena overflow at {name} ({nbytes} B); segs={self.segs}")


def _bf16(a):
    return np.asarray(a, dtype=np.float32).astype(ml_dtypes.bfloat16)


STAGES = ["A0", "POOL", "ATT", "A3", "B1", "B2", "all"]


def build_program(stop_after="all", nb=NB, debug=True):
    nc = bass.Bass("TRN2", target_bir_lowering=False)
    sc = Sched()
    stage_i = STAGES.index(stop_after)
    dbg_out = []

    def din(name, shape, dt=F32):
        return nc.dram_tensor(name, list(shape), dt, kind="ExternalInput").ap()

    x_d = din("x", [nb, S, D])
    mem_d = din("mem", [nb, MEMLEN, D])
    lnp_d = din("lnp", [8, D])
    ident_d = din("ident", [128, 128], BF16)
    identf_d = din("identf", [128, 128])
    shiftb_d = din("shiftb", [64, 128])
    w_in_d = din("w_in", [D, 4096])
    bgate_d = din("b_gate", [2048])
    wpg_d = din("w_pool_grp", [4, 128, 128])
    pscale_d = din("pool_scale", [512])
    wpu_d = din("w_pool_up", [512, D])
    wau_d = din("w_attn_up", [512, D])
    wout_d = din("w_mix_out", [D, D])
    wmq_d = din("w_mq", [D, 512])
    wmk_d = din("w_mk", [D, 512])
    wmv_d = din("w_mv", [D, 512])
    wmo_d = din("w_mo", [512, D])
    wr_d = din("w_router", [D, 36])
    br_d = din("b_router", [36])
    wg_d = din("w_gate", [NE, D, FF])
    wu_d = din("w_up", [NE, D, FF])
    wd_d = din("w_down", [NE, FF, D])
    biasT_d = din("biasT", [H, 2, 128, 128])
    b31_d = din("b31", [128, H])
    maskd_d = din("maskd", [128, 128])
    pastb_d = din("pastbias", [128, NT, 8])
    ownfix_d = din("ownfix", [128, NT, 8])
    ind_d = din("indrows", [8, S], BF16)
    invc_d = din("invc", [128, 4, 16])
    out_d = nc.dram_tensor("out", [nb, S, D], F32, kind="ExternalOutput").ap()

    A = Arena(nc)
    P = [nc.alloc_psum_tensor(f"psb{i}", [128, 1024], F32) for i in range(4)]

    def bank(i):
        return P[i // 2][:, (i % 2) * 512:(i % 2 + 1) * 512]

    def PT(i):
        return ("ps", i)

    def dma(q, out, in_, reads, writes, key, **kw):
        sc.add(q, "dma_start", dict(out=out, in_=in_, **kw), reads, writes, dma_key=key)

    def mm(out, lhsT, rhs, start, stop, reads, writes):
        sc.add("pe", "matmul", dict(out=out, lhsT=lhsT, rhs=rhs, start=start, stop=stop), reads, writes)

    def tr(out, in_, reads, writes):
        sc.add("pe", "transpose", dict(out=out, in_=in_, identity=ident[:, :]), list(reads) + [("ident",)], writes)

    def act(out, in_, func, reads, writes, bias=0.0, scale=1.0):
        sc.add("act", "activation", dict(out=out, in_=in_, func=func, bias=bias, scale=scale), reads, writes)

    def tt(eng, out, in0, in1, op, reads, writes):
        sc.add(eng, "tensor_tensor", dict(out=out, in0=in0, in1=in1, op=op), reads, writes)

    def ts(eng, out, in0, s1, s2, op0, op1, reads, writes):
        kw = dict(out=out, in0=in0, scalar1=s1, scalar2=s2, op0=op0)
        if op1 is not None:
            kw["op1"] = op1
        sc.add(eng, "tensor_scalar", kw, reads, writes)

    def stt(eng, out, in0, scalar, in1, op0, op1, reads, writes):
        sc.add(eng, "scalar_tensor_tensor", dict(out=out, in0=in0, scalar=scalar, in1=in1, op0=op0, op1=op1), reads, writes)

    def cp(eng, out, in_, reads, writes):
        if eng == "act":
            sc.add("act", "copy", dict(out=out, in_=in_), reads, writes)
        else:
            sc.add(eng, "tensor_copy", dict(out=out, in_=in_), reads, writes)

    def memset(eng, ap, val, writes):
        sc.add(eng, "memset", dict(ap=ap, constant=val), (), writes)

    def dump(name, ap, shape, dt, reads):
        if not debug:
            return
        d = nc.dram_tensor(name, list(shape), dt, kind="ExternalOutput").ap()
        dbg_out.append(name)
        dma("sp", d, ap, reads, [("dbg", name)], "dbg_" + name)

    ident = A.alloc("ident", [128, 128], BF16)
    identf = A.alloc("identf", [128, 128], F32)
    shiftb = A.alloc("shiftb", [64, 128], F32)
    lncol = A.alloc("lncol", [128, 8, KC], F32)
    epsc = A.alloc("epsc", [128, 1], F32)
    onesb = A.alloc("onesb", [128, 128], BF16)
    dma("sp", ident[:, :], ident_d[:, :], [], [("ident",)], "c0_6")
    dma("sp", identf[:, :], identf_d[:, :], [], [("identf",)], "c0_7")
    dma("sp", shiftb[:, :], shiftb_d[:, :], [], [("shiftb",)], "c0_8")
    dma("sp", lncol[:, :, :], lnp_d.rearrange("r (k p) -> p r k", p=128), [], [("lncol",)], "c0_lncol",
        allow_slow_non_contiguous=True)
    memset("pool", epsc[:, :], EPS, [("epsc",)])
    memset("pool", onesb[:, :], 1.0, [("onesb",)])

    def load_lnbc(dst, r0):
        dma("sp", dst[:, :, :], lnp_d[r0:r0 + 2, :].partition_broadcast(128), [], [("lnbc", r0)], f"lnbc{r0}")

    def layer_norm_stats(src_ap, src_tok, st_t, mv_t, tag):
        for g in range(2):
            sc.add("dve", "bn_stats", dict(out=st_t[:, g, :], in_=src_ap[:, g * 512:(g + 1) * 512]), [src_tok], [("st", tag)])
        sc.add("dve", "bn_aggr", dict(out=mv_t[:, 0:2], in_=st_t[:, :, :].rearrange("p g n -> p (g n)")), [("st", tag)], [("mv", tag)])
        act(mv_t[:, 2:3], mv_t[:, 1:2], AF.Sqrt, [("mv", tag), ("epsc",)], [("mv", tag)], bias=epsc[:, 0:1], scale=1.0)
        sc.add("dve", "reciprocal", dict(out=mv_t[:, 2:3], in_=mv_t[:, 2:3]), [("mv", tag)], [("mv", tag)])
        stt("dve", mv_t[:, 3:4], mv_t[:, 0:1], -1.0, mv_t[:, 2:3], ALU.mult, ALU.mult, [("mv", tag)], [("mv", tag)])

    def transposes_to_T(xn4, dstT, blk, grow, brow, xn_tok, dst_name):
        for kc in range(KC):
            bi = kc % 2
            pb = bank(bi).bitcast(BF16)
            for j in range(4):
                tr(pb[:, j * 128:(j + 1) * 128], xn4[j][:, kc * 128:(kc + 1) * 128], [(xn_tok, j)], [PT(bi)])
            act(dstT[:, kc, blk * 512:(blk + 1) * 512], pb[:, 0:512], AF.Identity, [PT(bi), ("lncol",)],
                [(dst_name, kc, blk)], bias=lncol[:, brow, kc:kc + 1], scale=lncol[:, grow, kc:kc + 1])

    WKC = "(kc p) n -> p kc n"

    acc = A.alloc("acc", [128, NT, D], F32)
    hT = A.alloc("hT", [128, KC, S], BF16)
    R_hT = (A.off - 32768, 32768)
    mT = A.alloc("mT", [128, KC, S], BF16)
    R_m = (A.off - 32768, 32768)
    ypg = A.alloc("ypg", [128, 4, S], BF16)
    R_x = (A.off - 16384, 16384)
    attnT = A.alloc("attnT", [128, 4, S], BF16)
    R_y = (A.off - 16384, 16384)
    R_tmp = (A.off, A.limit - A.off)

    class _Ph:
        def __init__(self):
            self.cur = None
        def phase(self, segs):
            self.cur = SegArena(nc, segs)
        def alloc(self, name, shape, dtype):
            return self.cur.alloc(name, shape, dtype)
        def mark(self):
            return 0
        def release(self, m):
            pass
    A = _Ph()

    for b in range(nb):
        mk_b = 0
        A.phase([R_tmp, R_m, R_x, R_y])

        mk = A.mark()
        lnbc = A.alloc("lnbc", [128, 2, D], F32)
        load_lnbc(lnbc, 0)
        xt = [A.alloc("xt", [128, D], F32) for _ in range(4)]
        xn = [A.alloc("xn", [128, D], BF16) for _ in range(4)]
        st = [A.alloc("st", [128, 2, 6], F32) for _ in range(4)]
        mv = [A.alloc("mv", [128, 4], F32) for _ in range(4)]
        for t in range(NT):
            s4, blk = t % 4, t // 4
            dma("sp", xt[s4][:, :], x_d[b, t * 128:(t + 1) * 128, :], [], [("xt", s4)], f"xt{s4}")
            layer_norm_stats(xt[s4], ("xt", s4), st[s4], mv[s4], s4)
            act(xn[s4][:, :], xt[s4][:, :], AF.Identity, [("xt", s4), ("mv", s4)], [("xn", s4)],
                bias=mv[s4][:, 3:4], scale=mv[s4][:, 2:3])
            act(acc[:, t, :], xt[s4][:, :], AF.Identity, [("xt", s4), ("mv", s4)], [("acc", t)],
                bias=mv[s4][:, 3:4], scale=mv[s4][:, 2:3])
            tt("pool", acc[:, t, :], acc[:, t, :], lnbc[:, 0, :], ALU.mult, [("acc", t), ("lnbc", 0)], [("acc", t)])
            if t >= 1:
                tt("dve", acc[:, t - 1, :], acc[:, t - 1, :], lnbc[:, 1, :], ALU.add, [("acc", t - 1), ("lnbc", 0)], [("acc", t - 1)])
            if s4 == 3:
                transposes_to_T(xn, hT, blk, 0, 1, "xn", "hT")
        tt("dve", acc[:, NT - 1, :], acc[:, NT - 1, :], lnbc[:, 1, :], ALU.add, [("acc", NT - 1), ("lnbc", 0)], [("acc", NT - 1)])
        A.release(mk)
        sc.barrier()
        hT_all = [("hT", kc, blk) for kc in range(KC) for blk in range(NQB)]
        if stage_i == 0:
            if b == 0:
                dump("dbg_hT", hT[:, :, :], [128, KC, S], BF16, hT_all)
            for t in range(NT):
                dma("sp", out_d[b, t * 128:(t + 1) * 128, :], acc[:, t, :], [("acc", t)], [("outdram", b, t)], "out")
            A.release(mk_b)
            sc.barrier()
            continue

        A.phase([R_tmp, R_m, R_y])
        mk = A.mark()
        w_u = A.alloc("w_u", [128, KC, 512], BF16)
        wpg = A.alloc("wpg", [128, 4, 128], BF16)
        pscol = A.alloc("pscol", [128, 4], F32)
        invc = A.alloc("invc", [128, 4, 16], F32)
        PADW = 16
        U2 = [A.alloc("U", [128, PADW + S], F32) for _ in range(2)]
        SA = A.alloc("SA", [128, PADW + S], F32)
        SB = A.alloc("SB", [128, PADW + S], F32)
        ypre2 = [A.alloc("ypre", [128, S], BF16) for _ in range(2)]
        tmp16 = A.alloc("tmp16", [128, 16], F32)
        dma("pool", w_u[:, :, :], w_in_d.rearrange(WKC, p=128)[:, :, 0:512], [], [("w_u",)], "w_u")
        dma("pool", wpg[:, :, :], wpg_d.rearrange("g c e -> c g e"), [], [("wpg",)], "wpg")
        dma("sp", pscol[:, :], pscale_d.rearrange("(g p) -> p g", p=128), [], [("pscol",)], "pscol",
            allow_slow_non_contiguous=True)
        dma("sp", invc[:, :, :], invc_d[:, :, :], [], [("invc",)], "invc")
        for buf, nm in ((U2[0], ("U", 0)), (U2[1], ("U", 1)), (SA, "SA"), (SB, "SB")):
            memset("pool", buf[:, 0:PADW], 0.0, [nm if isinstance(nm, tuple) else (nm,)])

        def pool_proj(g):
            U = U2[g % 2]
            for blk in range(NQB):
                bi = 2 + (blk % 2)
                for kc in range(KC):
                    mm(bank(bi), w_u[:, kc, g * 128:(g + 1) * 128], hT[:, kc, blk * 512:(blk + 1) * 512], kc == 0, kc == KC - 1,
                       [("w_u",), ("hT", kc, blk)], [PT(bi)])
                cp("act", U[:, PADW + blk * 512:PADW + (blk + 1) * 512], bank(bi), [PT(bi)], [("U", g % 2)])

        def pool_chain(g):
            w = 2 << g
            U, Un = U2[g % 2], ("U", g % 2)
            ypre, yn = ypre2[g % 2], ("ypre", g % 2)
            src, srcn = U, Un
            bufs = [(SA, ("SA",)), (SB, ("SB",))]
            sh, k = 1, 0
            while sh < w:
                dst, dstn = bufs[k % 2]
                tt("dve", dst[:, PADW:PADW + S], src[:, PADW:PADW + S], src[:, PADW - sh:PADW + S - sh], ALU.add,
                   [srcn], [dstn])
                src, srcn = dst, dstn
                sh *= 2
                k += 1
            stt("dve", ypre[:, :], src[:, PADW:PADW + S], 1.0 / w, U[:, PADW:PADW + S], ALU.mult, ALU.subtract,
                [srcn, Un], [yn])
            tt("dve", tmp16[:, :], src[:, PADW:PADW + 16], invc[:, g, :], ALU.mult, [srcn, ("invc",)], [("tmp16",)])
            tt("dve", ypre[:, 0:16], tmp16[:, :], U[:, PADW:PADW + 16], ALU.subtract, [("tmp16",), Un, yn], [yn])

        def pool_grp(g):
            ypre, yn = ypre2[g % 2], ("ypre", g % 2)
            for blk in range(NQB):
                bi = 4 + (blk % 2)
                mm(bank(bi), wpg[:, g, :], ypre[:, blk * 512:(blk + 1) * 512], True, True, [("wpg",), yn], [PT(bi)])
                act(ypg[:, g, blk * 512:(blk + 1) * 512], bank(bi), AF.Identity, [PT(bi), ("pscol",)], [("ypg", g, blk)],
                    scale=pscol[:, g:g + 1])

        pool_proj(0)
        for g in range(4):
            if g + 1 < 4:
                pool_proj(g + 1)
            pool_chain(g)
            pool_grp(g)
        A.release(mk)
        sc.barrier()
        ypg_all = [("ypg", g, blk) for g in range(4) for blk in range(NQB)]
        if stage_i == 1:
            if b == 0:
                dump("dbg_ypg", ypg[:, :, :], [128, 4, S], BF16, ypg_all)
            for t in range(NT):
                dma("sp", out_d[b, t * 128:(t + 1) * 128, :], acc[:, t, :], [("acc", t)], [("outdram", b, t)], "out")
            A.release(mk_b)
            sc.barrier()
            continue

        A.phase([R_tmp, R_m])
        mk = A.mark()
        w_qkv = A.alloc("w_qkv", [128, KC, 1536], BF16)
        for hp_ in range(4):
            for sec in (2, 0, 1):
                c0 = sec * 512 + hp_ * 128
                dma("pool", w_qkv[:, :, c0:c0 + 128], w_in_d.rearrange(WKC, p=128)[:, :, 512 + c0:512 + c0 + 128], [],
                    [("w_qkv", sec, hp_)], f"w_qkv{sec}_{hp_}")
        qa = [A.alloc("qa", [128, S], BF16) for _ in range(2)]
        ka = [A.alloc("ka", [128, S], BF16) for _ in range(2)]
        va2 = [A.alloc("va", [128, NT, 2, 128], BF16) for _ in range(2)]
        EMT = A.alloc("EMT", [128, H, 2, 128], BF16)
        b31 = A.alloc("b31", [128, H], F32)
        nb31 = A.alloc("nb31", [128, H], F32)
        maskd = A.alloc("maskd", [128, 128], F32)
        pastb = A.alloc("pastb", [128, NT, 8], F32)
        ownfx = A.alloc("ownfx", [128, NT, 8], F32)
        gm = A.alloc("gm", [128, NT, 8], F32)
        m8 = A.alloc("m8", [128, 8], F32)
        selt = A.alloc("selt", [128, NT, 8], F32)
        selp = A.alloc("selp", [128, NT, 72], BF16)
        kbar = A.alloc("kbar", [128, 8], F32)
        kbarb = A.alloc("kbarb", [128, 8], BF16)
        ebias2 = [A.alloc("ebias", [128, 128], F32) for _ in range(2)]
        ptb = [A.alloc("ptb", [128, 512], BF16) for _ in range(4)]
        sums = A.alloc("sums", [128, 512], F32)
        rsum = A.alloc("rsum", [128, 512], F32)
        dma("sp", b31[:, :], b31_d[:, :], [], [("b31",)], "attc1")
        dma("sp", maskd[:, :], maskd_d[:, :], [], [("maskd",)], "attc2")
        dma("sp", pastb[:, :, :], pastb_d[:, :, :], [], [("pastb",)], "attc3")
        dma("sp", ownfx[:, :, :], ownfix_d[:, :, :], [], [("ownfx",)], "attc4")
        for i in range(2):
            dma("sp", ka[i][64:72, :], ind_d[:, :], [], [("ka_ind", i)], f"attc5_{i}")
        ts("dve", nb31[:, :], b31[:, :], -1.0, None, ALU.mult, None, [("b31",)], [("nb31",)])
        for vb_ in va2:
            memset("pool", vb_[:, :, 0, 64:128], 1.0, [("va_ones",)])
            memset("pool", vb_[:, :, 1, 0:64], 1.0, [("va_ones",)])
        memset("pool", selp[:, :, :], 0.0, [("selp",)])
        def prepA(h):
            hp, hh = divmod(h, 2)
            vb = va2[hp % 2]
            if hh == 0:
                for t in range(NT):
                    bi = 6 + t % 2
                    for kc in range(KC):
                        mm(bank(bi)[:, 0:128], hT[:, kc, t * 128:(t + 1) * 128], w_qkv[:, kc, 1024 + hp * 128:1024 + (hp + 1) * 128],
                           kc == 0, kc == KC - 1, [("w_qkv", 2, hp), ("hT", kc, t // 4)], [PT(bi)])
                    cp("act", vb[:, t, 0, 0:64], bank(bi)[:, 0:64], [PT(bi), ("va_ones",)], [("va", hp % 2, t)])
                    cp("dve", vb[:, t, 1, 64:128], bank(bi)[:, 64:128], [PT(bi), ("va_ones",)], [("va", hp % 2, t)])
            for which, dst, dn in ((0, qa[hh], "qa"), (1, ka[hh], "ka")):
                for blk in range(NQB):
                    bi = 6 + (blk % 2)
                    c0 = which * 512 + h * 64
                    for kc in range(KC):
                        mm(bank(bi)[0:64, :], w_qkv[:, kc, c0:c0 + 64], hT[:, kc, blk * 512:(blk + 1) * 512],
                           kc == 0, kc == KC - 1, [("w_qkv", which, hp), ("hT", kc, blk)], [PT(bi)])
                    cp("act" if blk % 2 == 0 else "dve", dst[0:64, blk * 512:(blk + 1) * 512], bank(bi)[0:64, :],
                       [PT(bi)], [(dn, hh, blk)])
            ka_all = [("ka", hh, blk) for blk in range(NQB)]
            sc.add("dve", "tensor_reduce", dict(out=kbar[0:64, :], in_=ka[hh][0:64, :].rearrange("p (j l) -> p j l", l=256),
                                                axis=AX.X, op=ALU.add), ka_all, [("kbar",)])
            cp("dve", kbarb[0:64, :], kbar[0:64, :], [("kbar",)], [("kbarb",)])
            th = []

            def gate_mm():
                for t in range(NT):
                    mm(bank(6)[:, t * 8:(t + 1) * 8], qa[hh][0:64, t * 128:(t + 1) * 128], kbarb[0:64, :], True, True,
                       [("qa", hh, t // 4), ("kbarb",)], [PT(6)])
            th.append(gate_mm)
            th.append(lambda: tt("dve", gm[:, :, :], bank(6)[:, 0:128].rearrange("p (t j) -> p t j", j=8), pastb[:, :, :], ALU.add,
                                 [PT(6), ("pastb",)], [("gm",)]))
            for t in range(8, NT):
                def mx(t=t):
                    sc.add("dve", "max", dict(out=m8[:, :], in_=gm[:, t, :]), [("gm",)], [("m8",)])
                    ts("dve", selt[:, t, :], gm[:, t, :], m8[:, 2:3], None, ALU.is_ge, None, [("gm",), ("m8",)], [("selt",)])
                th.append(mx)

            def fin():
                ts("dve", selt[:, 8:, :], selt[:, 8:, :], -1.0, -NEG, ALU.add, ALU.mult, [("selt",)], [("selt",)])
                tt("dve", selp[:, 8:, 64:72], selt[:, 8:, :], ownfx[:, 8:, :], ALU.max, [("selt",), ("ownfx",), ("selp",)], [("selp",)])
            th.append(fin)
            return th

        def prepB(h):
            hh = h % 2
            pbs = bank(7).bitcast(BF16)
            for t in range(8, NT):
                tr(pbs[0:72, (t - 8) * 128:(t - 7) * 128], selp[:, t, :], [("selp",)], [PT(7)])
            memset("pool", qa[hh][64:72, 0:1024], 0.0, [("qa_sel", hh)])
            cp("act", qa[hh][64:72, 1024:2048], pbs[64:72, 0:1024], [PT(7), ("qa_sel", hh)], [("qa_sel", hh)])

        all_items = [(h, qb, kt) for h in range(H) for qb in range(NQB) for kt in range(4 * qb + 4)]
        NI = len(all_items)
        NPH = NI // H
        LA = 2

        def emit_norm(h, qb, ob):
            hp, hh = divmod(h, 2)
            q0 = qb * 512
            if hh == 0:
                mm(bank(3)[0:64, :], identf[64:128, 64:128], sums[64:128, :], True, True, [("identf",), ("sums",)], [PT(3)])
                sc.add("dve", "reciprocal", dict(out=rsum[0:64, :], in_=bank(3)[0:64, :]), [PT(3)], [("rsum",)])
                tt("dve", attnT[0:64, hp, q0:q0 + 512], bank(ob)[0:64, :], rsum[0:64, :], ALU.mult,
                   [PT(ob), ("rsum",)], [("attnT", hp, qb)])
            else:
                mm(bank(3)[:, :], shiftb[0:64, :], sums[0:64, :], True, True, [("shiftb",), ("sums",)], [PT(3)])
                sc.add("dve", "reciprocal", dict(out=rsum[64:128, :], in_=bank(3)[64:128, :]), [PT(3)], [("rsum",)])
                tt("dve", attnT[64:128, hp, q0:q0 + 512], bank(ob)[64:128, :], rsum[64:128, :], ALU.mult,
                   [PT(ob), ("rsum",)], [("attnT", hp, qb)])

        for f_ in prepA(0):
            f_()
        prepB(0)
        for h in range(H):
            for off in range(2):
                ek = (h * 2 + off) % 2
                eb = ebias2[ek]
                dma("sp", eb[:, :], biasT_d[h, off, :, :], [], [("ebias", ek)], f"ebias{ek}")
                act(eb[:, :], eb[:, :], AF.Exp, [("ebias", ek), ("nb31",)], [("ebias", ek)], bias=nb31[:, h:h + 1], scale=1.0)
                if off == 0:
                    tt("dve", EMT[:, h, off, :], eb[:, :], maskd[:, :], ALU.mult, [("ebias", ek), ("maskd",)], [("EMT", h)])
                else:
                    cp("dve", EMT[:, h, off, :], eb[:, :], [("ebias", ek)], [("EMT", h)])
        deferred = []
        pend = []
        for i in range(NI + LA):
            if i < NI:
                h, qb, kt = all_items[i]
                hp, hh = divmod(h, 2)
                if i % NPH == 0 and h + 1 < H:
                    deferred = prepA(h + 1) + [lambda h1=h + 1: prepB(h1)]
                c0 = max(0, kt - 4 * qb) * 128
                sb = i % 3
                ps_ = i % 4
                q0 = qb * 512
                mm(bank(sb)[:, c0:512], ka[hh][0:72, kt * 128:(kt + 1) * 128], qa[hh][0:72, q0 + c0:q0 + 512], True, True,
                   [("ka", hh, kt // 4), ("ka_ind", hh), ("qa", hh, qb), ("qa_sel", hh)], [PT(sb)])
                act(ptb[ps_][:, c0:512], bank(sb)[:, c0:512], AF.Exp, [PT(sb), ("b31",)], [("ptb", ps_)],
                    bias=b31[:, h:h + 1], scale=DH ** -0.5)
                for qi in range(4):
                    off = (4 * qb + qi) - kt
                    if off in (0, 1):
                        tt("dve", ptb[ps_][:, qi * 128:(qi + 1) * 128], ptb[ps_][:, qi * 128:(qi + 1) * 128],
                           EMT[:, h, off, :], ALU.mult, [("ptb", ps_), ("EMT", h)], [("ptb", ps_)])
                if i % NPH >= 3 and deferred:
                    deferred.pop(0)()
            j = i - LA
            if j >= 0:
                h2, qb, kt = all_items[j]
                hp2, hh2 = divmod(h2, 2)
                vb = va2[hp2 % 2]
                c0 = max(0, kt - 4 * qb) * 128
                ps_ = j % 4
                ob = 4 + (h2 * NQB + qb) % 2
                nkt = 4 * qb + 4
                mm(bank(ob)[:, c0:512], vb[:, kt, hh2, :], ptb[ps_][:, c0:512], kt == 0, kt == nkt - 1,
                   [("va", hp2 % 2, kt), ("va_ones",), ("ptb", ps_)], [PT(ob)])
                for ph, pq, pob in pend:
                    emit_norm(ph, pq, pob)
                pend = []
                if kt == nkt - 1:
                    if hh2 == 0:
                        cp("act", sums[64:128, :], bank(ob)[64:128, :], [PT(ob)], [("sums",)])
                    else:
                        cp("act", sums[0:64, :], bank(ob)[0:64, :], [PT(ob)], [("sums",)])
                    pend.append((h2, qb, ob))
        for ph, pq, pob in pend:
            emit_norm(ph, pq, pob)
        while deferred:
            deferred.pop(0)()
        A.release(mk)
        sc.barrier()
        attn_all = [("attnT", hp, qb) for hp in range(4) for qb in range(NQB)]
        if stage_i == 2:
            if b == 0:
                dump("dbg_attnT", attnT[:, :, :], [128, 4, S], BF16, attn_all)
            for t in range(NT):
                dma("sp", out_d[b, t * 128:(t + 1) * 128, :], acc[:, t, :], [("acc", t)], [("outdram", b, t)], "out")
            A.release(mk_b)
            sc.barrier()
            continue

        A.phase([R_tmp])
        mk = A.mark()
        bgcol = A.alloc("bgcol", [128, 16], F32)
        dma("sp", bgcol[:, :], bgate_d.rearrange("(c p) -> p c", p=128), [], [("bgcol",)], "bgcol", allow_slow_non_contiguous=True)
        wgl = [A.alloc("wgl", [128, KC, 2, 256], BF16) for _ in range(2)]
        wpu = [A.alloc("wpu", [128, 4, 256], BF16) for _ in range(2)]
        wau = [A.alloc("wau", [128, 4, 256], BF16) for _ in range(2)]
        G0 = [A.alloc("G0", [128, 512], F32) for _ in range(2)]
        G1 = [A.alloc("G1", [128, 512], F32) for _ in range(2)]
        M1 = [A.alloc("M1", [128, 512], F32) for _ in range(2)]
        M2 = [A.alloc("M2", [128, 512], F32) for _ in range(2)]

        def a3_load(qt):
            ws = qt % 2
            for gi in range(2):
                c0 = 2048 + gi * 1024 + qt * 256
                dma("pool", wgl[ws][:, :, gi, :], w_in_d.rearrange(WKC, p=128)[:, :, c0:c0 + 256], [], [("wgl", ws)], f"wgl{ws}")
            dma("pool", wpu[ws][:, :, :], wpu_d.rearrange("(g p) n -> p g n", p=128)[:, :, qt * 256:(qt + 1) * 256], [], [("wpu", ws)], f"wpu{ws}")
            dma("pool", wau[ws][:, :, :], wau_d.rearrange("(g p) n -> p g n", p=128)[:, :, qt * 256:(qt + 1) * 256], [], [("wau", ws)], f"wau{ws}")

        it = 0
        a3_load(0)
        a3_load(1)
        for qt in range(4):
            ws = qt % 2
            for dcl in range(2):
                dc = qt * 2 + dcl
                for blk in range(NQB):
                    s2 = it % 2
                    pb0 = 4 * s2
                    cs = slice(blk * 512, (blk + 1) * 512)
                    for gi in range(2):
                        for kc in range(KC):
                            mm(bank(pb0 + gi), wgl[ws][:, kc, gi, dcl * 128:(dcl + 1) * 128], hT[:, kc, cs], kc == 0, kc == KC - 1,
                               [("wgl", ws), ("hT", kc, blk)], [PT(pb0 + gi)])
                        act((G0 if gi == 0 else G1)[s2][:, :], bank(pb0 + gi), AF.Sigmoid, [PT(pb0 + gi), ("bgcol",)],
                            [("G", gi, s2)], bias=bgcol[:, gi * 8 + dc:gi * 8 + dc + 1], scale=1.0)
                    for g in range(4):
                        mm(bank(pb0 + 2), wpu[ws][:, g, dcl * 128:(dcl + 1) * 128], ypg[:, g, cs], g == 0, g == 3,
                           [("wpu", ws), ("ypg", g, blk)], [PT(pb0 + 2)])
                    for hp in range(4):
                        mm(bank(pb0 + 3), wau[ws][:, hp, dcl * 128:(dcl + 1) * 128], attnT[:, hp, cs], hp == 0, hp == 3,
                           [("wau", ws), ("attnT", hp, blk)], [PT(pb0 + 3)])
                    tt("dve", M1[s2][:, :], G0[s2][:, :], bank(pb0 + 2), ALU.mult, [("G", 0, s2), PT(pb0 + 2)], [("M1", s2)])
                    tt("dve", M2[s2][:, :], G1[s2][:, :], bank(pb0 + 3), ALU.mult, [("G", 1, s2), PT(pb0 + 3)], [("M2", s2)])
                    tt("pool", mT[:, dc, cs], M1[s2][:, :], M2[s2][:, :], ALU.add, [("M1", s2), ("M2", s2)], [("mT", dc, blk)])
                    it += 1
            if qt + 2 < 4:
                a3_load(qt + 2)
        A.release(mk)
        sc.barrier()
        mT_all = [("mT", dc, blk) for dc in range(KC) for blk in range(NQB)]
        if stage_i == 3:
            if b == 0:
                dump("dbg_mT", mT[:, :, :], [128, KC, S], BF16, mT_all)
            for t in range(NT):
                dma("sp", out_d[b, t * 128:(t + 1) * 128, :], acc[:, t, :], [("acc", t)], [("outdram", b, t)], "out")
            A.release(mk_b)
            sc.barrier()
            continue

        A.phase([R_tmp, R_x, R_y])
        mk = A.mark()
        lnbc = A.alloc("lnbc", [128, 2, D], F32)
        load_lnbc(lnbc, 2)
        wout = A.alloc("wout", [128, KC, D], BF16)
        for hf_ in range(2):
            dma("pool", wout[:, :, hf_ * 512:(hf_ + 1) * 512], wout_d.rearrange(WKC, p=128)[:, :, hf_ * 512:(hf_ + 1) * 512], [],
                [("wout", hf_)], f"wout{hf_}")
        r1 = [A.alloc("r1", [128, D], F32) for _ in range(4)]
        xn = [A.alloc("xn", [128, D], BF16) for _ in range(4)]
        st = [A.alloc("st", [128, 2, 6], F32) for _ in range(4)]
        mv = [A.alloc("mv", [128, 4], F32) for _ in range(4)]
        def b1_mm(t):
            blk = t // 4
            pt_ = 1 + t % 2
            for hf in range(2):
                for dc in range(KC):
                    mm(P[pt_][:, hf * 512:(hf + 1) * 512], mT[:, dc, t * 128:(t + 1) * 128], wout[:, dc, hf * 512:(hf + 1) * 512],
                       dc == 0, dc == KC - 1, [("mT", dc, blk), ("wout", hf)], [PT(2 * pt_ + hf)])

        def b1_post(t):
            s4 = t % 4
            pt_ = 1 + t % 2
            stt("dve", r1[s4][:, :], acc[:, t, :], ALPHA, P[pt_][:, :], ALU.mult, ALU.add,
                [("acc", t), PT(2 * pt_), PT(2 * pt_ + 1)], [("r1", s4)])
            layer_norm_stats(r1[s4], ("r1", s4), st[s4], mv[s4], s4)
            act(xn[s4][:, :], r1[s4][:, :], AF.Identity, [("r1", s4), ("mv", s4)], [("xn", s4)],
                bias=mv[s4][:, 3:4], scale=mv[s4][:, 2:3])
            act(acc[:, t, :], r1[s4][:, :], AF.Identity, [("r1", s4), ("mv", s4)], [("acc", t)],
                bias=mv[s4][:, 3:4], scale=mv[s4][:, 2:3])
            tt("pool", acc[:, t, :], acc[:, t, :], lnbc[:, 0, :], ALU.mult, [("acc", t), ("lnbc", 2)], [("acc", t)])
            tt("pool", acc[:, t, :], acc[:, t, :], lnbc[:, 1, :], ALU.add, [("acc", t), ("lnbc", 2)], [("acc", t)])

        done_mm = set()
        for blk in range(NQB):
            for ti in range(4):
                t = blk * 4 + ti
                if t not in done_mm:
                    b1_mm(t)
                b1_post(t)
            for t2 in (blk * 4 + 4, blk * 4 + 5):
                if t2 < NT:
                    b1_mm(t2)
                    done_mm.add(t2)
            transposes_to_T(xn, hT, blk, 2, 3, "xn", "hT")
        A.release(mk)
        sc.barrier()
        if stage_i == 4:
            for t in range(NT):
                dma("sp", out_d[b, t * 128:(t + 1) * 128, :], acc[:, t, :], [("acc", t)], [("outdram", b, t)], "out")
            A.release(mk_b)
            sc.barrier()
            continue

        A.phase([R_tmp, R_x, R_y])
        mk = A.mark()
        lnbc = A.alloc("lnbc", [128, 2, D], F32)
        load_lnbc(lnbc, 4)
        wq = A.alloc("wq", [128, KC, 512], BF16)
        A2 = SegArena(nc, [R_m])
        wk = A2.alloc("wk", [128, KC, 512], BF16)
        wv = A2.alloc("wv", [128, KC, 512], BF16)
        wo = A.alloc("wo", [128, MH, D], BF16)
        memb = A2.alloc("memb", [128, 2, D], BF16)
        memT = A2.alloc("memT", [128, KC, MEMLEN], BF16)
        kmT = A.alloc("kmT", [128, MH, MEMLEN], BF16)
        vm = A.alloc("vm", [128, 2, 512], BF16)
        dma("pool", memb[:, :, :], mem_d[b].rearrange("(t p) d -> p t d", p=128), [], [("memb",)], "memb")
        dma("pool", wk[:, :, :], wmk_d.rearrange(WKC, p=128), [], [("wk",)], "wk")
        dma("pool", wv[:, :, :], wmv_d.rearrange(WKC, p=128), [], [("wv",)], "wv")
        dma("pool", wq[:, :, :], wmq_d.rearrange(WKC, p=128), [], [("wq",)], "wq")
        dma("pool", wo[:, :, :], wmo_d.rearrange(WKC, p=128), [], [("wo",)], "wo")
        for kc in range(KC):
            bi = kc % 2
            pb = bank(bi).bitcast(BF16)
            for j in range(2):
                tr(pb[:, j * 128:(j + 1) * 128], memb[:, j, kc * 128:(kc + 1) * 128], [("memb",)], [PT(bi)])
            cp("act" if kc % 2 == 0 else "dve", memT[:, kc, :], pb[:, 0:256], [PT(bi)], [("memT", kc)])
        memT_all = [("memT", kc) for kc in range(KC)]
        for hm in range(MH):
            bi = 2 + hm % 2
            for kc in range(KC):
                mm(bank(bi)[:, 0:256], wk[:, kc, hm * 128:(hm + 1) * 128], memT[:, kc, :], kc == 0, kc == KC - 1,
                   [("wk",), ("memT", kc)], [PT(bi)])
            cp("act", kmT[:, hm, :], bank(bi)[:, 0:256], [PT(bi)], [("kmT",)])
        for j in range(2):
            bi = 4 + j
            for kc in range(KC):
                mm(bank(bi), memT[:, kc, j * 128:(j + 1) * 128], wv[:, kc, :], kc == 0, kc == KC - 1, [("wv",), ("memT", kc)], [PT(bi)])
            cp("dve", vm[:, j, :], bank(bi), [PT(bi)], [("vm",)])
        sc.barrier()
        q1T = A.alloc("q1T", [128, MH, 512], BF16)
        oT = A.alloc("oT", [128, MH, 512], BF16)
        pm = [A.alloc("pm", [128, 512], BF16) for _ in range(4)]
        rs2 = [A.alloc("rs2", [128, 512], F32) for _ in range(2)]
        r1 = [A.alloc("r2", [128, D], F32) for _ in range(4)]
        xn = [A.alloc("xn", [128, D], BF16) for _ in range(4)]
        st = [A.alloc("st", [128, 2, 6], F32) for _ in range(4)]
        mv = [A.alloc("mv", [128, 4], F32) for _ in range(4)]

        def b2_qproj(qb):
            cs = slice(qb * 512, (qb + 1) * 512)
            for hm in range(MH):
                bi = hm % 2
                for kc in range(KC):
                    mm(bank(bi), wq[:, kc, hm * 128:(hm + 1) * 128], hT[:, kc, cs], kc == 0, kc == KC - 1,
                       [("wq",), ("hT", kc, qb)], [PT(bi)])
                cp("act" if hm % 2 == 0 else "dve", q1T[:, hm, :], bank(bi), [PT(bi)], [("q1T", hm)])

        def b2_attn(qb):
            items = [(hm, j) for hm in range(MH) for j in range(2)]
            LA = 2
            for i in range(len(items) + LA):
                if i < len(items):
                    hm, j = items[i]
                    sb = i % 4
                    mm(bank(sb), kmT[:, hm, j * 128:(j + 1) * 128], q1T[:, hm, :], True, True, [("kmT",), ("q1T", hm)], [PT(sb)])
                    act(pm[sb][:, :], bank(sb), AF.Exp, [PT(sb)], [("pm", sb)], scale=128.0 ** -0.5)
                k = i - LA
                if k >= 0:
                    hm, j = items[k]
                    sb = k % 4
                    ob = 4 + 2 * (hm % 2)
                    mm(bank(ob), vm[:, j, hm * 128:(hm + 1) * 128], pm[sb][:, :], j == 0, j == 1, [("vm",), ("pm", sb)], [PT(ob)])
                    mm(bank(ob + 1), onesb[:, :], pm[sb][:, :], j == 0, j == 1, [("onesb",), ("pm", sb)], [PT(ob + 1)])
                    if j == 1:
                        sc.add("dve", "reciprocal", dict(out=rs2[hm % 2][:, :], in_=bank(ob + 1)), [PT(ob + 1)], [("rs2", hm % 2)])
                        tt("dve", oT[:, hm, :], bank(ob), rs2[hm % 2][:, :], ALU.mult, [PT(ob), ("rs2", hm % 2)], [("oT", hm)])

        def b2_out_ln(qb):
            for ti in range(4):
                t = qb * 4 + ti
                s4 = ti
                po = 3 if ti % 2 == 0 else 0
                for hf in range(2):
                    for hm in range(MH):
                        mm(P[po][:, hf * 512:(hf + 1) * 512], oT[:, hm, ti * 128:(ti + 1) * 128], wo[:, hm, hf * 512:(hf + 1) * 512],
                           hm == 0, hm == MH - 1, [("oT", hm), ("wo",)], [PT(2 * po + hf)])
                stt("dve", r1[s4][:, :], acc[:, t, :], ALPHA, P[po][:, :], ALU.mult, ALU.add,
                    [("acc", t), PT(2 * po), PT(2 * po + 1)], [("r1", s4)])
                layer_norm_stats(r1[s4], ("r1", s4), st[s4], mv[s4], s4)
                act(xn[s4][:, :], r1[s4][:, :], AF.Identity, [("r1", s4), ("mv", s4)], [("xn", s4)],
                    bias=mv[s4][:, 3:4], scale=mv[s4][:, 2:3])
                act(acc[:, t, :], r1[s4][:, :], AF.Identity, [("r1", s4), ("mv", s4)], [("acc", t)],
                    bias=mv[s4][:, 3:4], scale=mv[s4][:, 2:3])
                tt("pool", acc[:, t, :], acc[:, t, :], lnbc[:, 0, :], ALU.mult, [("acc", t), ("lnbc", 4)], [("acc", t)])
                tt("pool", acc[:, t, :], acc[:, t, :], lnbc[:, 1, :], ALU.add, [("acc", t), ("lnbc", 4)], [("acc", t)])

        b2_qproj(0)
        for qb in range(NQB):
            b2_attn(qb)
            if qb >= 1:
                transposes_to_T(xn, mT, qb - 1, 4, 5, "xn", "mT")
            if qb + 1 < NQB:
                b2_qproj(qb + 1)
            b2_out_ln(qb)
        transposes_to_T(xn, mT, NQB - 1, 4, 5, "xn", "mT")
        A.release(mk)
        sc.barrier()
        if stage_i == 5:
            for t in range(NT):
                dma("sp", out_d[b, t * 128:(t + 1) * 128, :], acc[:, t, :], [("acc", t)], [("outdram", b, t)], "out")
            A.release(mk_b)
            sc.barrier()
            continue

        A.phase([R_tmp, R_x, R_y, R_hT])
        mk = A.mark()
        lnbc = A.alloc("lnbc", [128, 2, D], F32)
        load_lnbc(lnbc, 6)
        wr = A.alloc("wr", [128, KC, 36], BF16)
        brb = A.alloc("brb", [128, 36], F32)
        dma("pool", wr[:, :, :], wr_d.rearrange(WKC, p=128), [], [("wr",)], "wr")
        dma("sp", brb[:, :], br_d.partition_broadcast(128), [], [("brb",)], "brb")
        comb = A.alloc("comb", [128, NT, NE], F32)
        lg = A.alloc("lg", [128, 36], F32)
        cm = A.alloc("cm", [128, 8], F32)
        ec = A.alloc("ec", [128, 4], F32)
        oh = A.alloc("oh", [128, 4], F32)
        ef = A.alloc("ef", [128, 4, 8], F32)
        fm = A.alloc("fm", [128, 4], F32)
        v8 = A.alloc("v8", [128, 4, 8], F32)
        sg = A.alloc("sg", [128, 4], F32)
        msk = A.alloc("msk", [128, 4, 8], F32)
        def router(t):
            for kc in range(KC):
                mm(bank(5)[:, 0:36], mT[:, kc, t * 128:(t + 1) * 128], wr[:, kc, :], kc == 0, kc == KC - 1,
                   [("mT", kc, t // 4), ("wr",)], [PT(5)])
            tt("dve", lg[:, :], bank(5)[:, 0:36], brb[:, :], ALU.add, [PT(5), ("brb",)], [("lg",)])
            sc.add("dve", "tensor_reduce", dict(out=cm[:, 0:1], in_=lg[:, 0:4], axis=AX.X, op=ALU.max), [("lg",)], [("cm",)])
            ts("dve", oh[:, :], lg[:, 0:4], cm[:, 0:1], None, ALU.is_ge, None, [("lg",), ("cm",)], [("oh",)])
            ts("dve", cm[:, 1:2], cm[:, 0:1], -1.0, None, ALU.mult, None, [("cm",)], [("cm",)])
            act(ec[:, :], lg[:, 0:4], AF.Exp, [("lg",), ("cm",)], [("ec",)], bias=cm[:, 1:2], scale=1.0)
            sc.add("dve", "tensor_reduce", dict(out=cm[:, 2:3], in_=ec[:, :], axis=AX.X, op=ALU.add), [("ec",)], [("cm",)])
            sc.add("dve", "reciprocal", dict(out=cm[:, 3:4], in_=cm[:, 2:3]), [("cm",)], [("cm",)])
            lf = lg[:, 4:36].rearrange("p (g j) -> p g j", j=8)
            sc.add("dve", "tensor_reduce", dict(out=fm[:, :], in_=lf, axis=AX.X, op=ALU.max), [("lg",)], [("fm",)])
            tt("dve", ef[:, :, :], lf, fm[:, :].unsqueeze(2).to_broadcast([128, 4, 8]), ALU.subtract, [("lg",), ("fm",)], [("ef",)])
            act(ef[:, :, :], ef[:, :, :], AF.Exp, [("ef",)], [("ef",)])
            for g in range(4):
                sc.add("dve", "max", dict(out=v8[:, g, :], in_=ef[:, g, :]), [("ef",)], [("v8",)])
            ts("dve", sg[:, :], v8[:, :, 1], 1.0, None, ALU.add, None, [("v8",)], [("sg",)])
            sc.add("dve", "reciprocal", dict(out=sg[:, :], in_=sg[:, :]), [("sg",)], [("sg",)])
            stt("dve", sg[:, :], sg[:, :], cm[:, 3:4], oh[:, :], ALU.mult, ALU.mult, [("sg",), ("cm",), ("oh",)], [("sg",)])
            tt("dve", msk[:, :, :], ef[:, :, :], v8[:, :, 1:2].to_broadcast([128, 4, 8]), ALU.is_ge, [("ef",), ("v8",)], [("msk",)])
            tt("dve", msk[:, :, :], msk[:, :, :], ef[:, :, :], ALU.mult, [("msk",), ("ef",)], [("msk",)])
            tt("dve", comb[:, t, :].rearrange("p (g j) -> p g j", j=8), msk[:, :, :],
               sg[:, :].unsqueeze(2).to_broadcast([128, 4, 8]), ALU.mult, [("msk",), ("sg",)], [("comb", t)])
        wgu = [A.alloc("wgu", [128, 2, KC, 512], BF16) for _ in range(2)]
        wdn = [A.alloc("wdn", [128, 2, 2, D], BF16) for _ in range(2)]
        sa = [[A.alloc("sa", [128, 256], F32) for _ in range(2)] for _ in range(2)]
        hid = [[A.alloc("hid", [128, 256], BF16) for _ in range(2)] for _ in range(2)]
        hidT = [A.alloc("hidT", [128, 2, 2, 128], BF16) for _ in range(2)]
        NP_ = NE // 2
        NU = NP_ * NT
        pb4 = P[2][:, 0:512].bitcast(BF16)

        def w_dma(pr):
            ws = pr % 2
            for ei in range(2):
                e = pr * 2 + ei
                dma("pool", wgu[ws][:, ei, :, 0:256], wg_d[e].rearrange(WKC, p=128), [], [("wgu", ws)], f"wgu{ws}")
                dma("pool", wgu[ws][:, ei, :, 256:512], wu_d[e].rearrange(WKC, p=128), [], [("wgu", ws)], f"wgu{ws}")
                dma("pool", wdn[ws][:, ei, :, :], wd_d[e].rearrange("(c p) d -> p c d", p=128), [], [("wdn", ws)], f"wdn{ws}")

        def u_up(u):
            pr, t = divmod(u, NT)
            ws, par = pr % 2, u % 2
            for kc in range(KC):
                for ei in range(2):
                    mm(bank(2 * par + ei), mT[:, kc, t * 128:(t + 1) * 128], wgu[ws][:, ei, kc, :], kc == 0, kc == KC - 1,
                       [("mT", kc, t // 4), ("wgu", ws)], [PT(2 * par + ei)])

        def u_elem(u):
            pr, t = divmod(u, NT)
            par = u % 2
            for ei in range(2):
                e = pr * 2 + ei
                bk = 2 * par + ei
                act(sa[par][ei][:, :], bank(bk)[:, 0:256], AF.Silu, [PT(bk)], [("sa", par, ei)])
                stt("dve", hid[par][ei][:, :], sa[par][ei][:, :], comb[:, t, e:e + 1], bank(bk)[:, 256:512], ALU.mult, ALU.mult,
                    [("sa", par, ei), ("comb", t), PT(bk)], [("hid", par, ei)])

        def u_tr(u):
            par = u % 2
            for ei in range(2):
                for c in range(2):
                    col = par * 512 + (ei * 2 + c) * 128
                    tr(pb4[:, col:col + 128], hid[par][ei][:, c * 128:(c + 1) * 128], [("hid", par, ei)], [("ps4", par)])
            cp("act", hidT[par][:, :, :, :].rearrange("p e c t -> p (e c t)"), pb4[:, par * 512:(par + 1) * 512],
               [("ps4", par)], [("hidT", par)])

        def u_down(u):
            pr, t = divmod(u, NT)
            ws, par = pr % 2, u % 2
            for hf in range(2):
                k = 0
                for ei in range(2):
                    for c in range(2):
                        mm(P[3][:, hf * 512:(hf + 1) * 512], hidT[par][:, ei, c, :], wdn[ws][:, ei, c, hf * 512:(hf + 1) * 512],
                           k == 0, k == 3, [("hidT", par), ("wdn", ws)], [PT(6 + hf)])
                        k += 1
            if pr == 0:
                stt("dve", acc[:, t, :], acc[:, t, :], ALPHA, P[3][:, :], ALU.mult, ALU.add,
                    [("acc", t), PT(6), PT(7)], [("acc", t)])
            else:
                tt("dve", acc[:, t, :], acc[:, t, :], P[3][:, :], ALU.add, [("acc", t), PT(6), PT(7)], [("acc", t)])

        st3 = [A.alloc("st", [128, 2, 6], F32) for _ in range(2)]
        mv3 = [A.alloc("mv", [128, 4], F32) for _ in range(2)]
        ob_ = [A.alloc("ob", [128, D], F32) for _ in range(2)]

        def ln3_store(t):
            s2 = t % 2
            layer_norm_stats(acc[:, t, :], ("acc", t), st3[s2], mv3[s2], ("ln3", s2))
            act(ob_[s2][:, :], acc[:, t, :], AF.Identity, [("acc", t), ("mv", ("ln3", s2))], [("ob", s2)],
                bias=mv3[s2][:, 3:4], scale=mv3[s2][:, 2:3])
            tt("pool", ob_[s2][:, :], ob_[s2][:, :], lnbc[:, 0, :], ALU.mult, [("ob", s2), ("lnbc", 6)], [("ob", s2)])
            tt("pool", ob_[s2][:, :], ob_[s2][:, :], lnbc[:, 1, :], ALU.add, [("ob", s2), ("lnbc", 6)], [("ob", s2)])
            dma("sp", out_d[b, t * 128:(t + 1) * 128, :], ob_[s2][:, :], [("ob", s2)], [("outdram", b, t), ("ob", s2)], f"out{s2}")

        w_dma(0)
        w_dma(1)
        for t_ in range(3):
            router(t_)
        u_up(0)
        u_elem(0)
        for u in range(NU):
            pr, t = divmod(u, NT)
            if t == 0 and pr >= 1 and pr + 1 < NP_:
                w_dma(pr + 1)
            u_tr(u)
            if u + 1 < NU:
                u_up(u + 1)
                u_elem(u + 1)
            u_down(u)
            if pr == 0 and t + 3 < NT:
                router(t + 3)
            if pr == NP_ - 1:
                ln3_store(t)
        A.release(mk)
        A.release(mk_b)
        sc.barrier()

    fin = [("outdram", b, t) for b in range(nb) for t in range(NT)] + [("dbg", n) for n in dbg_out]
    sc.add("sp", "nop", dict(), fin, [])

    sc.finalize()
    keys = set()
    for op in sc.ops:
        for k, v in op.waits:
            keys.add(k)
        if op.dma_key is not None:
            keys.add(("dma", op.dma_key))
        elif op.sig:
            keys.add(("eng", op.eng))
    keys = sorted(keys)
    from contextlib import ExitStack
    with ExitStack() as es:
        sems = {k: es.enter_context(nc.semaphore(f"s_{k[0]}_{k[1]}")) for k in keys}
        block = es.enter_context(nc.Block())

        @block.tensor
        def _(e):
            sc.emit(nc, "pe", e, sems)

        @block.scalar
        def _(e):
            sc.emit(nc, "act", e, sems)

        @block.vector
        def _(e):
            sc.emit(nc, "dve", e, sems)

        @block.gpsimd
        def _(e):
            sc.emit(nc, "pool", e, sems)

        @block.sync
        def _(e):
            sc.emit(nc, "sp", e, sems)
    return nc, dbg_out, dict(n_ops=len(sc.ops), sbuf_peak=0, n_sems=len(keys))


def _rel_bucket_np(dist):
    d = np.maximum(dist, 0)
    large = 16 + (np.log(np.maximum(d, 1).astype(np.float32) / np.float32(16)) / np.float32(math.log(128 / 16))
                  * np.float32(16)).astype(np.int32)
    large = np.minimum(large, 31)
    return np.where(d < 16, d, large)


def make_inputs(inputs, nb=NB):
    f = lambda k: np.ascontiguousarray(np.asarray(inputs[k], dtype=np.float32))
    x = f("x")
    mem = f("mem")
    lnp = np.stack([f("ln_in_g").reshape(-1), f("ln_in_b").reshape(-1), f("ln1_g").reshape(-1), f("ln1_b").reshape(-1),
                    f("ln2_g").reshape(-1), f("ln2_b").reshape(-1), f("ln3_g").reshape(-1), f("ln3_b").reshape(-1)])
    rel = f("rel_bias")
    kk = np.arange(128)[:, None]
    qq = np.arange(128)[None, :]
    biasT = np.empty((H, 2, 128, 128), np.float32)
    for off in range(2):
        bk = _rel_bucket_np(off * 128 + qq - kk)
        biasT[:, off] = rel[bk].transpose(2, 0, 1)
    b31 = np.ascontiguousarray(np.broadcast_to(rel[31][None, :], (128, H)))
    maskd = (qq >= kk).astype(np.float32)
    tq = np.arange(NT)[:, None] // 2
    jj = np.arange(8)[None, :]
    pastbias = np.where(jj < tq, 0.0, -1e30).astype(np.float32)
    ownfix = np.where(jj == tq, 0.0, -1e9).astype(np.float32)
    pastbias = np.ascontiguousarray(np.broadcast_to(pastbias[None], (128, NT, 8)))
    ownfix = np.ascontiguousarray(np.broadcast_to(ownfix[None], (128, NT, 8)))
    ind = (np.arange(S)[None, :] // 256 == np.arange(8)[:, None]).astype(np.float32).astype(ml_dtypes.bfloat16)
    tt_ = np.arange(16)[None, :]
    ww = np.array([2, 4, 8, 16])[:, None]
    invc = (1.0 / np.minimum(tt_ + 1, ww)).astype(np.float32)
    invc = np.ascontiguousarray(np.broadcast_to(invc[None], (128, 4, 16)))
    shiftb = np.zeros((64, 128), np.float32)
    shiftb[np.arange(64), 64 + np.arange(64)] = 1.0
    common = {
        "lnp": lnp, "ident": np.eye(128, dtype=np.float32).astype(ml_dtypes.bfloat16), "identf": np.eye(128, dtype=np.float32),
        "shiftb": shiftb, "w_in": f("w_in")[0], "b_gate": f("b_gate")[0], "w_pool_grp": f("w_pool_grp")[0],
        "pool_scale": f("pool_scale")[0], "w_pool_up": f("w_pool_up")[0], "w_attn_up": f("w_attn_up")[0],
        "w_mix_out": f("w_mix_out")[0], "w_mq": f("w_mq")[0], "w_mk": f("w_mk")[0], "w_mv": f("w_mv")[0], "w_mo": f("w_mo")[0],
        "w_router": np.ascontiguousarray(np.concatenate([f("w_coarse")[0], f("w_fine")[0].reshape(D, 32)], axis=1)),
        "b_router": np.ascontiguousarray(np.concatenate([f("b_coarse")[0], f("b_fine")[0].reshape(32)])),
        "w_gate": f("w_gate")[0], "w_up": f("w_up")[0], "w_down": f("w_down")[0],
        "biasT": biasT, "b31": b31, "maskd": maskd, "pastbias": pastbias, "ownfix": ownfix, "indrows": ind, "invc": invc,
    }
    maps = []
    for c in range(NCORES):
        m = dict(common)
        m["x"] = x[c * nb:(c + 1) * nb]
        m["mem"] = mem[c * nb:(c + 1) * nb]
        maps.append(m)
    return maps


_CACHE = {}


def kernel(**inputs):
    if "nc" not in _CACHE:
        _CACHE["nc"] = build_program("all", debug=False)
    nc, _, _ = _CACHE["nc"]
    maps = make_inputs(inputs)
    res = run_bass_kernel_spmd(nc, maps, core_ids=list(range(NCORES)))
    outs = [np.asarray(r["out"]) for r in res.results]
    return np.concatenate(outs, axis=0).astype(np.float32)
```

```python
import math
import numpy as np
import ml_dtypes
import concourse.bass as bass
import concourse.mybir as mybir
from concourse.bass_utils import run_bass_kernel_spmd

F32 = mybir.dt.float32
BF16 = mybir.dt.bfloat16
AF = mybir.ActivationFunctionType
ALU = mybir.AluOpType
AX = mybir.AxisListType

D = 1024
S = 2048
NB = 2
NCORES = 8
KC = D // 128
NT = S // 128
NQB = S // 512
H = 8
DH = 64
MEMLEN = 256
MH = 4
NE = 32
FF = 256
ALPHA = 2.0 ** 0.25
EPS = 1e-5
NEG = -30000.0


class _Op:
    __slots__ = ("eng", "fn", "deps", "idx", "sig", "sigval", "dma_key", "dma_val", "waits")


class Sched:
    def __init__(self, same_engine_sync=True):
        self.ops = []
        self.lastw = {}
        self.readers = {}
        self.dma_count = {}
        self.same_engine_sync = same_engine_sync

    def add(self, eng, meth, kw, reads=(), writes=(), dma_key=None):
        op = _Op()
        op.eng, op.fn, op.idx, op.dma_key = eng, (meth, kw), len(self.ops), dma_key
        op.sig, op.sigval, op.dma_val, op.waits = False, 0, 0, []
        deps = set()
        for t in reads:
            w = self.lastw.get(t)
            if w is not None:
                deps.add(w)
        for t in writes:
            w = self.lastw.get(t)
            if w is not None:
                deps.add(w)
            for r in self.readers.get(t, ()):
                deps.add(r)
        deps.discard(op.idx)
        op.deps = deps
        for t in reads:
            self.readers.setdefault(t, []).append(op.idx)
        for t in writes:
            self.lastw[t] = op.idx
            self.readers[t] = []
        if dma_key is not None:
            self.dma_count[dma_key] = self.dma_count.get(dma_key, 0) + 1
            op.dma_val = 16 * self.dma_count[dma_key]
        self.ops.append(op)
        return op.idx

    def barrier(self, engines=("pe", "act", "dve", "pool", "sp")):
        last = {}
        for op in self.ops:
            if op.dma_key is not None:
                last[("dma", op.dma_key)] = op.idx
            else:
                last[("eng", op.eng)] = op.idx
        deps = set(last.values())
        for e in engines:
            op = _Op()
            op.eng, op.fn, op.idx, op.dma_key = e, ("nop", {}), len(self.ops), None
            op.sig, op.sigval, op.dma_val, op.waits = False, 0, 0, []
            op.deps = set(deps)
            self.ops.append(op)

    def finalize(self):
        ops = self.ops
        for op in ops:
            best = {}
            for d in op.deps:
                p = ops[d]
                if p.dma_key is not None:
                    continue
                if p.eng == op.eng and (p.eng == "pe" or not self.same_engine_sync) and op.dma_key is None:
                    continue
                if d > best.get(p.eng, -1):
                    best[p.eng] = d
            for d in best.values():
                ops[d].sig = True
        cnt = {}
        for op in ops:
            if op.sig:
                cnt[op.eng] = cnt.get(op.eng, 0) + 1
                op.sigval = cnt[op.eng]
        waited = {}
        for op in ops:
            need = {}
            for d in op.deps:
                p = ops[d]
                if p.dma_key is not None:
                    k, v = ("dma", p.dma_key), p.dma_val
                else:
                    if not p.sig:
                        continue
                    if p.eng == op.eng and (p.eng == "pe" or not self.same_engine_sync) and op.dma_key is None:
                        continue
                    k, v = ("eng", p.eng), p.sigval
                if v > need.get(k, 0):
                    need[k] = v
            w = waited.setdefault(op.eng, {})
            for k, v in need.items():
                if v > w.get(k, 0):
                    w[k] = v
                    op.waits.append((k, v))

    def emit(self, nc, eng_name, eng, sems):
        n = 0
        for op in self.ops:
            if op.eng != eng_name:
                continue
            for k, v in op.waits:
                eng.wait_ge(sems[k], v)
            ins = getattr(eng, op.fn[0])(**op.fn[1])
            if op.dma_key is not None:
                ins.then_inc(sems[("dma", op.dma_key)], 16)
            elif op.sig:
                ins.then_inc(sems[("eng", op.eng)], 1)
            n += 1
        return n


class Arena:
    def __init__(self, nc, start=16640, limit=224 * 1024):
        self.nc, self.off, self.limit, self.n = nc, start, limit, 0
        self.peak = 0

    def alloc(self, name, shape, dtype):
        nbytes = int(np.prod(shape[1:])) * (4 if dtype == F32 else 2)
        nbytes = (nbytes + 63) // 64 * 64
        self.n += 1
        t = self.nc.alloc_sbuf_tensor_at(f"{name}_{self.n}", list(shape), dtype, offset=self.off)
        self.off += nbytes
        self.peak = max(self.peak, self.off)
        assert self.off <= self.limit, f"SBUF arena overflow at {name}: {self.off}"
        return t

    def mark(self):
        return self.off

    def release(self, m):
        self.off = m


class SegArena:
    _n = [0]

    def __init__(self, nc, segs):
        self.nc = nc
        self.segs = [[a, a, a + sz] for a, sz in segs]

    def alloc(self, name, shape, dtype):
        nbytes = int(np.prod(shape[1:])) * (4 if dtype == F32 else 2)
        nbytes = (nbytes + 63) // 64 * 64
        for sg in self.segs:
            if sg[1] + nbytes <= sg[2]:
                SegArena._n[0] += 1
                t = self.nc.alloc_sbuf_tensor_at(f"{name}_s{SegArena._n[0]}", list(shape), dtype, offset=sg[1])
                sg[1] += nbytes
                return t
        raise AssertionError(f"SegArena overflow at {name} ({nbytes} B); segs={self.segs}")


def _bf16(a):
    return np.asarray(a, dtype=np.float32).astype(ml_dtypes.bfloat16)


STAGES = ["A0", "POOL", "ATT", "A3", "B1", "B2", "all"]


def build_program(stop_after="all", nb=NB, debug=True):
    nc = bass.Bass("TRN2", target_bir_lowering=False)
    sc = Sched()
    stage_i = STAGES.index(stop_after)
    dbg_out = []

    def din(name, shape, dt=F32):
        return nc.dram_tensor(name, list(shape), dt, kind="ExternalInput").ap()

    x_d = din("x", [nb, S, D])
    mem_d = din("mem", [nb, MEMLEN, D])
    lnp_d = din("lnp", [8, D])
    ident_d = din("ident", [128, 128], BF16)
    identf_d = din("identf", [128, 128])
    shiftb_d = din("shiftb", [64, 128])
    w_in_d = din("w_in", [D, 4096])
    bgate_d = din("b_gate", [2048])
    wpg_d = din("w_pool_grp", [4, 128, 128])
    pscale_d = din("pool_scale", [512])
    wpu_d = din("w_pool_up", [512, D])
    wau_d = din("w_attn_up", [512, D])
    wout_d = din("w_mix_out", [D, D])
    wmq_d = din("w_mq", [D, 512])
    wmk_d = din("w_mk", [D, 512])
    wmv_d = din("w_mv", [D, 512])
    wmo_d = din("w_mo", [512, D])
    wr_d = din("w_router", [D, 36])
    br_d = din("b_router", [36])
    wg_d = din("w_gate", [NE, D, FF])
    wu_d = din("w_up", [NE, D, FF])
    wd_d = din("w_down", [NE, FF, D])
    biasT_d = din("biasT", [H, 2, 128, 128])
    b31_d = din("b31", [128, H])
    maskd_d = din("maskd", [128, 128])
    pastb_d = din("pastbias", [128, NT, 8])
    ownfix_d = din("ownfix", [128, NT, 8])
    ind_d = din("indrows", [8, S], BF16)
    invc_d = din("invc", [128, 4, 16])
    out_d = nc.dram_tensor("out", [nb, S, D], F32, kind="ExternalOutput").ap()

    A = Arena(nc)
    P = [nc.alloc_psum_tensor(f"psb{i}", [128, 1024], F32) for i in range(4)]

    def bank(i):
        return P[i // 2][:, (i % 2) * 512:(i % 2 + 1) * 512]

    def PT(i):
        return ("ps", i)

    def dma(q, out, in_, reads, writes, key, **kw):
        sc.add(q, "dma_start", dict(out=out, in_=in_, **kw), reads, writes, dma_key=key)

    def mm(out, lhsT, rhs, start, stop, reads, writes):
        sc.add("pe", "matmul", dict(out=out, lhsT=lhsT, rhs=rhs, start=start, stop=stop), reads, writes)

    def tr(out, in_, reads, writes):
        sc.add("pe", "transpose", dict(out=out, in_=in_, identity=ident[:, :]), list(reads) + [("ident",)], writes)

    def act(out, in_, func, reads, writes, bias=0.0, scale=1.0):
        sc.add("act", "activation", dict(out=out, in_=in_, func=func, bias=bias, scale=scale), reads, writes)

    def tt(eng, out, in0, in1, op, reads, writes):
        sc.add(eng, "tensor_tensor", dict(out=out, in0=in0, in1=in1, op=op), reads, writes)

    def ts(eng, out, in0, s1, s2, op0, op1, reads, writes):
        kw = dict(out=out, in0=in0, scalar1=s1, scalar2=s2, op0=op0)
        if op1 is not None:
            kw["op1"] = op1
        sc.add(eng, "tensor_scalar", kw, reads, writes)

    def stt(eng, out, in0, scalar, in1, op0, op1, reads, writes):
        sc.add(eng, "scalar_tensor_tensor", dict(out=out, in0=in0, scalar=scalar, in1=in1, op0=op0, op1=op1), reads, writes)

    def cp(eng, out, in_, reads, writes):
        if eng == "act":
            sc.add("act", "copy", dict(out=out, in_=in_), reads, writes)
        else:
            sc.add(eng, "tensor_copy", dict(out=out, in_=in_), reads, writes)

    def memset(eng, ap, val, writes):
        sc.add(eng, "memset", dict(ap=ap, constant=val), (), writes)

    def dump(name, ap, shape, dt, reads):
        if not debug:
            return
        d = nc.dram_tensor(name, list(shape), dt, kind="ExternalOutput").ap()
        dbg_out.append(name)
        dma("sp", d, ap, reads, [("dbg", name)], "dbg_" + name)

    ident = A.alloc("ident", [128, 128], BF16)
    identf = A.alloc("identf", [128, 128], F32)
    shiftb = A.alloc("shiftb", [64, 128], F32)
    lncol = A.alloc("lncol", [128, 8, KC], F32)
    epsc = A.alloc("epsc", [128, 1], F32)
    onesb = A.alloc("onesb", [128, 128], BF16)
    dma("sp", ident[:, :], ident_d[:, :], [], [("ident",)], "c0_6")
    dma("sp", identf[:, :], identf_d[:, :], [], [("identf",)], "c0_7")
    dma("sp", shiftb[:, :], shiftb_d[:, :], [], [("shiftb",)], "c0_8")
    dma("sp", lncol[:, :, :], lnp_d.rearrange("r (k p) -> p r k", p=128), [], [("lncol",)], "c0_lncol",
        allow_slow_non_contiguous=True)
    memset("pool", epsc[:, :], EPS, [("epsc",)])
    memset("pool", onesb[:, :], 1.0, [("onesb",)])

    def load_lnbc(dst, r0):
        dma("sp", dst[:, :, :], lnp_d[r0:r0 + 2, :].partition_broadcast(128), [], [("lnbc", r0)], f"lnbc{r0}")

    def layer_norm_stats(src_ap, src_tok, st_t, mv_t, tag):
        for g in range(2):
            sc.add("dve", "bn_stats", dict(out=st_t[:, g, :], in_=src_ap[:, g * 512:(g + 1) * 512]), [src_tok], [("st", tag)])
        sc.add("dve", "bn_aggr", dict(out=mv_t[:, 0:2], in_=st_t[:, :, :].rearrange("p g n -> p (g n)")), [("st", tag)], [("mv", tag)])
        act(mv_t[:, 2:3], mv_t[:, 1:2], AF.Sqrt, [("mv", tag), ("epsc",)], [("mv", tag)], bias=epsc[:, 0:1], scale=1.0)
        sc.add("dve", "reciprocal", dict(out=mv_t[:, 2:3], in_=mv_t[:, 2:3]), [("mv", tag)], [("mv", tag)])
        stt("dve", mv_t[:, 3:4], mv_t[:, 0:1], -1.0, mv_t[:, 2:3], ALU.mult, ALU.mult, [("mv", tag)], [("mv", tag)])

    def transposes_to_T(xn4, dstT, blk, grow, brow, xn_tok, dst_name):
        for kc in range(KC):
            bi = kc % 2
            pb = bank(bi).bitcast(BF16)
            for j in range(4):
                tr(pb[:, j * 128:(j + 1) * 128], xn4[j][:, kc * 128:(kc + 1) * 128], [(xn_tok, j)], [PT(bi)])
            act(dstT[:, kc, blk * 512:(blk + 1) * 512], pb[:, 0:512], AF.Identity, [PT(bi), ("lncol",)],
                [(dst_name, kc, blk)], bias=lncol[:, brow, kc:kc + 1], scale=lncol[:, grow, kc:kc + 1])

    WKC = "(kc p) n -> p kc n"

    acc = A.alloc("acc", [128, NT, D], F32)
    hT = A.alloc("hT", [128, KC, S], BF16)
    R_hT = (A.off - 32768, 32768)
    mT = A.alloc("mT", [128, KC, S], BF16)
    R_m = (A.off - 32768, 32768)
    ypg = A.alloc("ypg", [128, 4, S], BF16)
    R_x = (A.off - 16384, 16384)
    attnT = A.alloc("attnT", [128, 4, S], BF16)
    R_y = (A.off - 16384, 16384)
    R_tmp = (A.off, A.limit - A.off)

    class _Ph:
        def __init__(self):
            self.cur = None
        def phase(self, segs):
            self.cur = SegArena(nc, segs)
        def alloc(self, name, shape, dtype):
            return self.cur.alloc(name, shape, dtype)
        def mark(self):
            return 0
        def release(self, m):
            pass
    A = _Ph()

    for b in range(nb):
        mk_b = 0
        A.phase([R_tmp, R_m, R_x, R_y])

        mk = A.mark()
        lnbc = A.alloc("lnbc", [128, 2, D], F32)
        load_lnbc(lnbc, 0)
        xt = [A.alloc("xt", [128, D], F32) for _ in range(4)]
        xn = [A.alloc("xn", [128, D], BF16) for _ in range(4)]
        st = [A.alloc("st", [128, 2, 6], F32) for _ in range(4)]
        mv = [A.alloc("mv", [128, 4], F32) for _ in range(4)]
        for t in range(NT):
            s4, blk = t % 4, t // 4
            dma("sp", xt[s4][:, :], x_d[b, t * 128:(t + 1) * 128, :], [], [("xt", s4)], f"xt{s4}")
            layer_norm_stats(xt[s4], ("xt", s4), st[s4], mv[s4], s4)
            act(xn[s4][:, :], xt[s4][:, :], AF.Identity, [("xt", s4), ("mv", s4)], [("xn", s4)],
                bias=mv[s4][:, 3:4], scale=mv[s4][:, 2:3])
            act(acc[:, t, :], xt[s4][:, :], AF.Identity, [("xt", s4), ("mv", s4)], [("acc", t)],
                bias=mv[s4][:, 3:4], scale=mv[s4][:, 2:3])
            tt("pool", acc[:, t, :], acc[:, t, :], lnbc[:, 0, :], ALU.mult, [("acc", t), ("lnbc", 0)], [("acc", t)])
            if t >= 1:
                tt("dve", acc[:, t - 1, :], acc[:, t - 1, :], lnbc[:, 1, :], ALU.add, [("acc", t - 1), ("lnbc", 0)], [("acc", t - 1)])
            if s4 == 3:
                transposes_to_T(xn, hT, blk, 0, 1, "xn", "hT")
        tt("dve", acc[:, NT - 1, :], acc[:, NT - 1, :], lnbc[:, 1, :], ALU.add, [("acc", NT - 1), ("lnbc", 0)], [("acc", NT - 1)])
        A.release(mk)
        sc.barrier()
        hT_all = [("hT", kc, blk) for kc in range(KC) for blk in range(NQB)]
        if stage_i == 0:
            if b == 0:
                dump("dbg_hT", hT[:, :, :], [128, KC, S], BF16, hT_all)
            for t in range(NT):
                dma("sp", out_d[b, t * 128:(t + 1) * 128, :], acc[:, t, :], [("acc", t)], [("outdram", b, t)], "out")
            A.release(mk_b)
            sc.barrier()
            continue

        A.phase([R_tmp, R_m, R_y])
        mk = A.mark()
        w_u = A.alloc("w_u", [128, KC, 512], BF16)
        wpg = A.alloc("wpg", [128, 4, 128], BF16)
        pscol = A.alloc("pscol", [128, 4], F32)
        invc = A.alloc("invc", [128, 4, 16], F32)
        PADW = 16
        U2 = [A.alloc("U", [128, PADW + S], F32) for _ in range(2)]
        SA = A.alloc("SA", [128, PADW + S], F32)
        SB = A.alloc("SB", [128, PADW + S], F32)
        ypre2 = [A.alloc("ypre", [128, S], BF16) for _ in range(2)]
        tmp16 = A.alloc("tmp16", [128, 16], F32)
        dma("pool", w_u[:, :, :], w_in_d.rearrange(WKC, p=128)[:, :, 0:512], [], [("w_u",)], "w_u")
        dma("pool", wpg[:, :, :], wpg_d.rearrange("g c e -> c g e"), [], [("wpg",)], "wpg")
        dma("sp", pscol[:, :], pscale_d.rearrange("(g p) -> p g", p=128), [], [("pscol",)], "pscol",
            allow_slow_non_contiguous=True)
        dma("sp", invc[:, :, :], invc_d[:, :, :], [], [("invc",)], "invc")
        for buf, nm in ((U2[0], ("U", 0)), (U2[1], ("U", 1)), (SA, "SA"), (SB, "SB")):
            memset("pool", buf[:, 0:PADW], 0.0, [nm if isinstance(nm, tuple) else (nm,)])

        def pool_proj(g):
            U = U2[g % 2]
            for blk in range(NQB):
                bi = 2 + (blk % 2)
                for kc in range(KC):
                    mm(bank(bi), w_u[:, kc, g * 128:(g + 1) * 128], hT[:, kc, blk * 512:(blk + 1) * 512], kc == 0, kc == KC - 1,
                       [("w_u",), ("hT", kc, blk)], [PT(bi)])
                cp("act", U[:, PADW + blk * 512:PADW + (blk + 1) * 512], bank(bi), [PT(bi)], [("U", g % 2)])

        def pool_chain(g):
            w = 2 << g
            U, Un = U2[g % 2], ("U", g % 2)
            ypre, yn = ypre2[g % 2], ("ypre", g % 2)
            src, srcn = U, Un
            bufs = [(SA, ("SA",)), (SB, ("SB",))]
            sh, k = 1, 0
            while sh < w:
                dst, dstn = bufs[k % 2]
                tt("dve", dst[:, PADW:PADW + S], src[:, PADW:PADW + S], src[:, PADW - sh:PADW + S - sh], ALU.add,
                   [srcn], [dstn])
                src, srcn = dst, dstn
                sh *= 2
                k += 1
            stt("dve", ypre[:, :], src[:, PADW:PADW + S], 1.0 / w, U[:, PADW:PADW + S], ALU.mult, ALU.subtract,
                [srcn, Un], [yn])
            tt("dve", tmp16[:, :], src[:, PADW:PADW + 16], invc[:, g, :], ALU.mult, [srcn, ("invc",)], [("tmp16",)])
            tt("dve", ypre[:, 0:16], tmp16[:, :], U[:, PADW:PADW + 16], ALU.subtract, [("tmp16",), Un, yn], [yn])

        def pool_grp(g):
            ypre, yn = ypre2[g % 2], ("ypre", g % 2)
            for blk in range(NQB):
                bi = 4 + (blk % 2)
                mm(bank(bi), wpg[:, g, :], ypre[:, blk * 512:(blk + 1) * 512], True, True, [("wpg",), yn], [PT(bi)])
                act(ypg[:, g, blk * 512:(blk + 1) * 512], bank(bi), AF.Identity, [PT(bi), ("pscol",)], [("ypg", g, blk)],
                    scale=pscol[:, g:g + 1])

        pool_proj(0)
        for g in range(4):
            if g + 1 < 4:
                pool_proj(g + 1)
            pool_chain(g)
            pool_grp(g)
        A.release(mk)
        sc.barrier()
        ypg_all = [("ypg", g, blk) for g in range(4) for blk in range(NQB)]
        if stage_i == 1:
            if b == 0:
                dump("dbg_ypg", ypg[:, :, :], [128, 4, S], BF16, ypg_all)
            for t in range(NT):
                dma("sp", out_d[b, t * 128:(t + 1) * 128, :], acc[:, t, :], [("acc", t)], [("outdram", b, t)], "out")
            A.release(mk_b)
            sc.barrier()
            continue

        A.phase([R_tmp, R_m])
        mk = A.mark()
        w_qkv = A.alloc("w_qkv", [128, KC, 1536], BF16)
        for hp_ in range(4):
            for sec in (2, 0, 1):
                c0 = sec * 512 + hp_ * 128
                dma("pool", w_qkv[:, :, c0:c0 + 128], w_in_d.rearrange(WKC, p=128)[:, :, 512 + c0:512 + c0 + 128], [],
                    [("w_qkv", sec, hp_)], f"w_qkv{sec}_{hp_}")
        qa = [A.alloc("qa", [128, S], BF16) for _ in range(2)]
        ka = [A.alloc("ka", [128, S], BF16) for _ in range(2)]
        va2 = [A.alloc("va", [128, NT, 2, 128], BF16) for _ in range(2)]
        EMT = A.alloc("EMT", [128, H, 2, 128], BF16)
        b31 = A.alloc("b31", [128, H], F32)
        nb31 = A.alloc("nb31", [128, H], F32)
        maskd = A.alloc("maskd", [128, 128], F32)
        pastb = A.alloc("pastb", [128, NT, 8], F32)
        ownfx = A.alloc("ownfx", [128, NT, 8], F32)
        gm = A.alloc("gm", [128, NT, 8], F32)
        m8 = A.alloc("m8", [128, 8], F32)
        selt = A.alloc("selt", [128, NT, 8], F32)
        selp = A.alloc("selp", [128, NT, 72], BF16)
        kbar = A.alloc("kbar", [128, 8], F32)
        kbarb = A.alloc("kbarb", [128, 8], BF16)
        ebias2 = [A.alloc("ebias", [128, 128], F32) for _ in range(2)]
        ptb = [A.alloc("ptb", [128, 512], BF16) for _ in range(4)]
        sums = A.alloc("sums", [128, 512], F32)
        rsum = A.alloc("rsum", [128, 512], F32)
        dma("sp", b31[:, :], b31_d[:, :], [], [("b31",)], "attc1")
        dma("sp", maskd[:, :], maskd_d[:, :], [], [("maskd",)], "attc2")
        dma("sp", pastb[:, :, :], pastb_d[:, :, :], [], [("pastb",)], "attc3")
        dma("sp", ownfx[:, :, :], ownfix_d[:, :, :], [], [("ownfx",)], "attc4")
        for i in range(2):
            dma("sp", ka[i][64:72, :], ind_d[:, :], [], [("ka_ind", i)], f"attc5_{i}")
        ts("dve", nb31[:, :], b31[:, :], -1.0, None, ALU.mult, None, [("b31",)], [("nb31",)])
        for vb_ in va2:
            memset("pool", vb_[:, :, 0, 64:128], 1.0, [("va_ones",)])
            memset("pool", vb_[:, :, 1, 0:64], 1.0, [("va_ones",)])
        memset("pool", selp[:, :, :], 0.0, [("selp",)])
        def prepA(h):
            hp, hh = divmod(h, 2)
            vb = va2[hp % 2]
            if hh == 0:
                for t in range(NT):
                    bi = 6 + t % 2
                    for kc in range(KC):
                        mm(bank(bi)[:, 0:128], hT[:, kc, t * 128:(t + 1) * 128], w_qkv[:, kc, 1024 + hp * 128:1024 + (hp + 1) * 128],
                           kc == 0, kc == KC - 1, [("w_qkv", 2, hp), ("hT", kc, t // 4)], [PT(bi)])
                    cp("act", vb[:, t, 0, 0:64], bank(bi)[:, 0:64], [PT(bi), ("va_ones",)], [("va", hp % 2, t)])
                    cp("dve", vb[:, t, 1, 64:128], bank(bi)[:, 64:128], [PT(bi), ("va_ones",)], [("va", hp % 2, t)])
            for which, dst, dn in ((0, qa[hh], "qa"), (1, ka[hh], "ka")):
                for blk in range(NQB):
                    bi = 6 + (blk % 2)
                    c0 = which * 512 + h * 64
                    for kc in range(KC):
                        mm(bank(bi)[0:64, :], w_qkv[:, kc, c0:c0 + 64], hT[:, kc, blk * 512:(blk + 1) * 512],
                           kc == 0, kc == KC - 1, [("w_qkv", which, hp), ("hT", kc, blk)], [PT(bi)])
                    cp("act" if blk % 2 == 0 else "dve", dst[0:64, blk * 512:(blk + 1) * 512], bank(bi)[0:64, :],
                       [PT(bi)], [(dn, hh, blk)])
            ka_all = [("ka", hh, blk) for blk in range(NQB)]
            sc.add("dve", "tensor_reduce", dict(out=kbar[0:64, :], in_=ka[hh][0:64, :].rearrange("p (j l) -> p j l", l=256),
                                                axis=AX.X, op=ALU.add), ka_all, [("kbar",)])
            cp("dve", kbarb[0:64, :], kbar[0:64, :], [("kbar",)], [("kbarb",)])
            th = []

            def gate_mm():
                for t in range(NT):
                    mm(bank(6)[:, t * 8:(t + 1) * 8], qa[hh][0:64, t * 128:(t + 1) * 128], kbarb[0:64, :], True, True,
                       [("qa", hh, t // 4), ("kbarb",)], [PT(6)])
            th.append(gate_mm)
            th.append(lambda: tt("dve", gm[:, :, :], bank(6)[:, 0:128].rearrange("p (t j) -> p t j", j=8), pastb[:, :, :], ALU.add,
                                 [PT(6), ("pastb",)], [("gm",)]))
            for t in range(8, NT):
                def mx(t=t):
                    sc.add("dve", "max", dict(out=m8[:, :], in_=gm[:, t, :]), [("gm",)], [("m8",)])
                    ts("dve", selt[:, t, :], gm[:, t, :], m8[:, 2:3], None, ALU.is_ge, None, [("gm",), ("m8",)], [("selt",)])
                th.append(mx)

            def fin():
                ts("dve", selt[:, 8:, :], selt[:, 8:, :], -1.0, -NEG, ALU.add, ALU.mult, [("selt",)], [("selt",)])
                tt("dve", selp[:, 8:, 64:72], selt[:, 8:, :], ownfx[:, 8:, :], ALU.max, [("selt",), ("ownfx",), ("selp",)], [("selp",)])
            th.append(fin)
            return th

        def prepB(h):
            hh = h % 2
            pbs = bank(7).bitcast(BF16)
            for t in range(8, NT):
                tr(pbs[0:72, (t - 8) * 128:(t - 7) * 128], selp[:, t, :], [("selp",)], [PT(7)])
            memset("pool", qa[hh][64:72, 0:1024], 0.0, [("qa_sel", hh)])
            cp("act", qa[hh][64:72, 1024:2048], pbs[64:72, 0:1024], [PT(7), ("qa_sel", hh)], [("qa_sel", hh)])

        all_items = [(h, qb, kt) for h in range(H) for qb in range(NQB) for kt in range(4 * qb + 4)]
        NI = len(all_items)
        NPH = NI // H
        LA = 2

        def emit_norm(h, qb, ob):
            hp, hh = divmod(h, 2)
            q0 = qb * 512
            if hh == 0:
                mm(bank(3)[0:64, :], identf[64:128, 64:128], sums[64:128, :], True, True, [("identf",), ("sums",)], [PT(3)])
                sc.add("dve", "reciprocal", dict(out=rsum[0:64, :], in_=bank(3)[0:64, :]), [PT(3)], [("rsum",)])
                tt("dve", attnT[0:64, hp, q0:q0 + 512], bank(ob)[0:64, :], rsum[0:64, :], ALU.mult,
                   [PT(ob), ("rsum",)], [("attnT", hp, qb)])
            else:
                mm(bank(3)[:, :], shiftb[0:64, :], sums[0:64, :], True, True, [("shiftb",), ("sums",)], [PT(3)])
                sc.add("dve", "reciprocal", dict(out=rsum[64:128, :], in_=bank(3)[64:128, :]), [PT(3)], [("rsum",)])
                tt("dve", attnT[64:128, hp, q0:q0 + 512], bank(ob)[64:128, :], rsum[64:128, :], ALU.mult,
                   [PT(ob), ("rsum",)], [("attnT", hp, qb)])

        for f_ in prepA(0):
            f_()
        prepB(0)
        for h in range(H):
            for off in range(2):
                ek = (h * 2 + off) % 2
                eb = ebias2[ek]
                dma("sp", eb[:, :], biasT_d[h, off, :, :], [], [("ebias", ek)], f"ebias{ek}")
                act(eb[:, :], eb[:, :], AF.Exp, [("ebias", ek), ("nb31",)], [("ebias", ek)], bias=nb31[:, h:h + 1], scale=1.0)
                if off == 0:
                    tt("dve", EMT[:, h, off, :], eb[:, :], maskd[:, :], ALU.mult, [("ebias", ek), ("maskd",)], [("EMT", h)])
                else:
                    cp("dve", EMT[:, h, off, :], eb[:, :], [("ebias", ek)], [("EMT", h)])
        deferred = []
        pend = []
        for i in range(NI + LA):
            if i < NI:
                h, qb, kt = all_items[i]
                hp, hh = divmod(h, 2)
                if i % NPH == 0 and h + 1 < H:
                    deferred = prepA(h + 1) + [lambda h1=h + 1: prepB(h1)]
                c0 = max(0, kt - 4 * qb) * 128
                sb = i % 3
                ps_ = i % 4
                q0 = qb * 512
                mm(bank(sb)[:, c0:512], ka[hh][0:72, kt * 128:(kt + 1) * 128], qa[hh][0:72, q0 + c0:q0 + 512], True, True,
                   [("ka", hh, kt // 4), ("ka_ind", hh), ("qa", hh, qb), ("qa_sel", hh)], [PT(sb)])
                act(ptb[ps_][:, c0:512], bank(sb)[:, c0:512], AF.Exp, [PT(sb), ("b31",)], [("ptb", ps_)],
                    bias=b31[:, h:h + 1], scale=DH ** -0.5)
                for qi in range(4):
                    off = (4 * qb + qi) - kt
                    if off in (0, 1):
                        tt("dve", ptb[ps_][:, qi * 128:(qi + 1) * 128], ptb[ps_][:, qi * 128:(qi + 1) * 128],
                           EMT[:, h, off, :], ALU.mult, [("ptb", ps_), ("EMT", h)], [("ptb", ps_)])
                if i % NPH >= 3 and deferred:
                    deferred.pop(0)()
            j = i - LA
            if j >= 0:
                h2, qb, kt = all_items[j]
                hp2, hh2 = divmod(h2, 2)
                vb = va2[hp2 % 2]
                c0 = max(0, kt - 4 * qb) * 128
                ps_ = j % 4
                ob = 4 + (h2 * NQB + qb) % 2
                nkt = 4 * qb + 4
                mm(bank(ob)[:, c0:512], vb[:, kt, hh2, :], ptb[ps_][:, c0:512], kt == 0, kt == nkt - 1,
                   [("va", hp2 % 2, kt), ("va_ones",), ("ptb", ps_)], [PT(ob)])
                for ph, pq, pob in pend:
                    emit_norm(ph, pq, pob)
                pend = []
                if kt == nkt - 1:
                    if hh2 == 0:
                        cp("act", sums[64:128, :], bank(ob)[64:128, :], [PT(ob)], [("sums",)])
                    else:
                        cp("act", sums[0:64, :], bank(ob)[0:64, :], [PT(ob)], [("sums",)])
                    pend.append((h2, qb, ob))
        for ph, pq, pob in pend:
            emit_norm(ph, pq, pob)
        while deferred:
            deferred.pop(0)()
        A.release(mk)
        sc.barrier()
        attn_all = [("attnT", hp, qb) for hp in range(4) for qb in range(NQB)]
        if stage_i == 2:
            if b == 0:
                dump("dbg_attnT", attnT[:, :, :], [128, 4, S], BF16, attn_all)
            for t in range(NT):
                dma("sp", out_d[b, t * 128:(t + 1) * 128, :], acc[:, t, :], [("acc", t)], [("outdram", b, t)], "out")
            A.release(mk_b)
            sc.barrier()
            continue

        A.phase([R_tmp])
        mk = A.mark()
        bgcol = A.alloc("bgcol", [128, 16], F32)
        dma("sp", bgcol[:, :], bgate_d.rearrange("(c p) -> p c", p=128), [], [("bgcol",)], "bgcol", allow_slow_non_contiguous=True)
        wgl = [A.alloc("wgl", [128, KC, 2, 256], BF16) for _ in range(2)]
        wpu = [A.alloc("wpu", [128, 4, 256], BF16) for _ in range(2)]
        wau = [A.alloc("wau", [128, 4, 256], BF16) for _ in range(2)]
        G0 = [A.alloc("G0", [128, 512], F32) for _ in range(2)]
        G1 = [A.alloc("G1", [128, 512], F32) for _ in range(2)]
        M1 = [A.alloc("M1", [128, 512], F32) for _ in range(2)]
        M2 = [A.alloc("M2", [128, 512], F32) for _ in range(2)]

        def a3_load(qt):
            ws = qt % 2
            for gi in range(2):
                c0 = 2048 + gi * 1024 + qt * 256
                dma("pool", wgl[ws][:, :, gi, :], w_in_d.rearrange(WKC, p=128)[:, :, c0:c0 + 256], [], [("wgl", ws)], f"wgl{ws}")
            dma("pool", wpu[ws][:, :, :], wpu_d.rearrange("(g p) n -> p g n", p=128)[:, :, qt * 256:(qt + 1) * 256], [], [("wpu", ws)], f"wpu{ws}")
            dma("pool", wau[ws][:, :, :], wau_d.rearrange("(g p) n -> p g n", p=128)[:, :, qt * 256:(qt + 1) * 256], [], [("wau", ws)], f"wau{ws}")

        it = 0
        a3_load(0)
        a3_load(1)
        for qt in range(4):
            ws = qt % 2
            for dcl in range(2):
                dc = qt * 2 + dcl
                for blk in range(NQB):
                    s2 = it % 2
                    pb0 = 4 * s2
                    cs = slice(blk * 512, (blk + 1) * 512)
                    for gi in range(2):
                        for kc in range(KC):
                            mm(bank(pb0 + gi), wgl[ws][:, kc, gi, dcl * 128:(dcl + 1) * 128], hT[:, kc, cs], kc == 0, kc == KC - 1,
                               [("wgl", ws), ("hT", kc, blk)], [PT(pb0 + gi)])
                        act((G0 if gi == 0 else G1)[s2][:, :], bank(pb0 + gi), AF.Sigmoid, [PT(pb0 + gi), ("bgcol",)],
                            [("G", gi, s2)], bias=bgcol[:, gi * 8 + dc:gi * 8 + dc + 1], scale=1.0)
                    for g in range(4):
                        mm(bank(pb0 + 2), wpu[ws][:, g, dcl * 128:(dcl + 1) * 128], ypg[:, g, cs], g == 0, g == 3,
                           [("wpu", ws), ("ypg", g, blk)], [PT(pb0 + 2)])
                    for hp in range(4):
                        mm(bank(pb0 + 3), wau[ws][:, hp, dcl * 128:(dcl + 1) * 128], attnT[:, hp, cs], hp == 0, hp == 3,
                           [("wau", ws), ("attnT", hp, blk)], [PT(pb0 + 3)])
                    tt("dve", M1[s2][:, :], G0[s2][:, :], bank(pb0 + 2), ALU.mult, [("G", 0, s2), PT(pb0 + 2)], [("M1", s2)])
                    tt("dve", M2[s2][:, :], G1[s2][:, :], bank(pb0 + 3), ALU.mult, [("G", 1, s2), PT(pb0 + 3)], [("M2", s2)])
                    tt("pool", mT[:, dc, cs], M1[s2][:, :], M2[s2][:, :], ALU.add, [("M1", s2), ("M2", s2)], [("mT", dc, blk)])
                    it += 1
            if qt + 2 < 4:
                a3_load(qt + 2)
        A.release(mk)
        sc.barrier()
        mT_all = [("mT", dc, blk) for dc in range(KC) for blk in range(NQB)]
        if stage_i == 3:
            if b == 0:
                dump("dbg_mT", mT[:, :, :], [128, KC, S], BF16, mT_all)
            for t in range(NT):
                dma("sp", out_d[b, t * 128:(t + 1) * 128, :], acc[:, t, :], [("acc", t)], [("outdram", b, t)], "out")
            A.release(mk_b)
            sc.barrier()
            continue

        A.phase([R_tmp, R_x, R_y])
        mk = A.mark()
        lnbc = A.alloc("lnbc", [128, 2, D], F32)
        load_lnbc(lnbc, 2)
        wout = A.alloc("wout", [128, KC, D], BF16)
        for hf_ in range(2):
            dma("pool", wout[:, :, hf_ * 512:(hf_ + 1) * 512], wout_d.rearrange(WKC, p=128)[:, :, hf_ * 512:(hf_ + 1) * 512], [],
                [("wout", hf_)], f"wout{hf_}")
        r1 = [A.alloc("r1", [128, D], F32) for _ in range(4)]
        xn = [A.alloc("xn", [128, D], BF16) for _ in range(4)]
        st = [A.alloc("st", [128, 2, 6], F32) for _ in range(4)]
        mv = [A.alloc("mv", [128, 4], F32) for _ in range(4)]
        def b1_mm(t):
            blk = t // 4
            pt_ = 1 + t % 2
            for hf in range(2):
                for dc in range(KC):
                    mm(P[pt_][:, hf * 512:(hf + 1) * 512], mT[:, dc, t * 128:(t + 1) * 128], wout[:, dc, hf * 512:(hf + 1) * 512],
                       dc == 0, dc == KC - 1, [("mT", dc, blk), ("wout", hf)], [PT(2 * pt_ + hf)])

        def b1_post(t):
            s4 = t % 4
            pt_ = 1 + t % 2
            stt("dve", r1[s4][:, :], acc[:, t, :], ALPHA, P[pt_][:, :], ALU.mult, ALU.add,
                [("acc", t), PT(2 * pt_), PT(2 * pt_ + 1)], [("r1", s4)])
            layer_norm_stats(r1[s4], ("r1", s4), st[s4], mv[s4], s4)
            act(xn[s4][:, :], r1[s4][:, :], AF.Identity, [("r1", s4), ("mv", s4)], [("xn", s4)],
                bias=mv[s4][:, 3:4], scale=mv[s4][:, 2:3])
            act(acc[:, t, :], r1[s4][:, :], AF.Identity, [("r1", s4), ("mv", s4)], [("acc", t)],
                bias=mv[s4][:, 3:4], scale=mv[s4][:, 2:3])
            tt("pool", acc[:, t, :], acc[:, t, :], lnbc[:, 0, :], ALU.mult, [("acc", t), ("lnbc", 2)], [("acc", t)])
            tt("pool", acc[:, t, :], acc[:, t, :], lnbc[:, 1, :], ALU.add, [("acc", t), ("lnbc", 2)], [("acc", t)])

        done_mm = set()
        for blk in range(NQB):
            for ti in range(4):
                t = blk * 4 + ti
                if t not in done_mm:
                    b1_mm(t)
                b1_post(t)
            for t2 in (blk * 4 + 4, blk * 4 + 5):
                if t2 < NT:
                    b1_mm(t2)
                    done_mm.add(t2)
            transposes_to_T(xn, hT, blk, 2, 3, "xn", "hT")
        A.release(mk)
        sc.barrier()
        if stage_i == 4:
            for t in range(NT):
                dma("sp", out_d[b, t * 128:(t + 1) * 128, :], acc[:, t, :], [("acc", t)], [("outdram", b, t)], "out")
            A.release(mk_b)
            sc.barrier()
            continue

        A.phase([R_tmp, R_x, R_y])
        mk = A.mark()
        lnbc = A.alloc("lnbc", [128, 2, D], F32)
        load_lnbc(lnbc, 4)
        wq = A.alloc("wq", [128, KC, 512], BF16)
        A2 = SegArena(nc, [R_m])
        wk = A2.alloc("wk", [128, KC, 512], BF16)
        wv = A2.alloc("wv", [128, KC, 512], BF16)
        wo = A.alloc("wo", [128, MH, D], BF16)
        memb = A2.alloc("memb", [128, 2, D], BF16)
        memT = A2.alloc("memT", [128, KC, MEMLEN], BF16)
        kmT = A.alloc("kmT", [128, MH, MEMLEN], BF16)
        vm = A.alloc("vm", [128, 2, 512], BF16)
        dma("pool", memb[:, :, :], mem_d[b].rearrange("(t p) d -> p t d", p=128), [], [("memb",)], "memb")
        dma("pool", wk[:, :, :], wmk_d.rearrange(WKC, p=128), [], [("wk",)], "wk")
        dma("pool", wv[:, :, :], wmv_d.rearrange(WKC, p=128), [], [("wv",)], "wv")
        dma("pool", wq[:, :, :], wmq_d.rearrange(WKC, p=128), [], [("wq",)], "wq")
        dma("pool", wo[:, :, :], wmo_d.rearrange(WKC, p=128), [], [("wo",)], "wo")
        for kc in range(KC):
            bi = kc % 2
            pb = bank(bi).bitcast(BF16)
            for j in range(2):
                tr(pb[:, j * 128:(j + 1) * 128], memb[:, j, kc * 128:(kc + 1) * 128], [("memb",)], [PT(bi)])
            cp("act" if kc % 2 == 0 else "dve", memT[:, kc, :], pb[:, 0:256], [PT(bi)], [("memT", kc)])
        memT_all = [("memT", kc) for kc in range(KC)]
        for hm in range(MH):
            bi = 2 + hm % 2
            for kc in range(KC):
                mm(bank(bi)[:, 0:256], wk[:, kc, hm * 128:(hm + 1) * 128], memT[:, kc, :], kc == 0, kc == KC - 1,
                   [("wk",), ("memT", kc)], [PT(bi)])
            cp("act", kmT[:, hm, :], bank(bi)[:, 0:256], [PT(bi)], [("kmT",)])
        for j in range(2):
            bi = 4 + j
            for kc in range(KC):
                mm(bank(bi), memT[:, kc, j * 128:(j + 1) * 128], wv[:, kc, :], kc == 0, kc == KC - 1, [("wv",), ("memT", kc)], [PT(bi)])
            cp("dve", vm[:, j, :], bank(bi), [PT(bi)], [("vm",)])
        sc.barrier()
        q1T = A.alloc("q1T", [128, MH, 512], BF16)
        oT = A.alloc("oT", [128, MH, 512], BF16)
        pm = [A.alloc("pm", [128, 512], BF16) for _ in range(4)]
        rs2 = [A.alloc("rs2", [128, 512], F32) for _ in range(2)]
        r1 = [A.alloc("r2", [128, D], F32) for _ in range(4)]
        xn = [A.alloc("xn", [128, D], BF16) for _ in range(4)]
        st = [A.alloc("st", [128, 2, 6], F32) for _ in range(4)]
        mv = [A.alloc("mv", [128, 4], F32) for _ in range(4)]

        def b2_qproj(qb):
            cs = slice(qb * 512, (qb + 1) * 512)
            for hm in range(MH):
                bi = hm % 2
                for kc in range(KC):
                    mm(bank(bi), wq[:, kc, hm * 128:(hm + 1) * 128], hT[:, kc, cs], kc == 0, kc == KC - 1,
                       [("wq",), ("hT", kc, qb)], [PT(bi)])
                cp("act" if hm % 2 == 0 else "dve", q1T[:, hm, :], bank(bi), [PT(bi)], [("q1T", hm)])

        def b2_attn(qb):
            items = [(hm, j) for hm in range(MH) for j in range(2)]
            LA = 2
            for i in range(len(items) + LA):
                if i < len(items):
                    hm, j = items[i]
                    sb = (i + 2) % 4
                    mm(bank(sb), kmT[:, hm, j * 128:(j + 1) * 128], q1T[:, hm, :], True, True, [("kmT",), ("q1T", hm)], [PT(sb)])
                    act(pm[sb][:, :], bank(sb), AF.Exp, [PT(sb)], [("pm", sb)], scale=128.0 ** -0.5)
                k = i - LA
                if k >= 0:
                    hm, j = items[k]
                    sb = (k + 2) % 4
                    ob = 4 + 2 * (hm % 2)
                    mm(bank(ob), vm[:, j, hm * 128:(hm + 1) * 128], pm[sb][:, :], j == 0, j == 1, [("vm",), ("pm", sb)], [PT(ob)])
                    mm(bank(ob + 1), onesb[:, :], pm[sb][:, :], j == 0, j == 1, [("onesb",), ("pm", sb)], [PT(ob + 1)])
                    if j == 1:
                        sc.add("dve", "reciprocal", dict(out=rs2[hm % 2][:, :], in_=bank(ob + 1)), [PT(ob + 1)], [("rs2", hm % 2)])
                        tt("dve", oT[:, hm, :], bank(ob), rs2[hm % 2][:, :], ALU.mult, [PT(ob), ("rs2", hm % 2)], [("oT", hm)])

        def b2_out_ln(qb):
            for ti in range(4):
                t = qb * 4 + ti
                s4 = ti
                po = 3 if ti % 2 == 0 else 0
                for hf in range(2):
                    for hm in range(MH):
                        mm(P[po][:, hf * 512:(hf + 1) * 512], oT[:, hm, ti * 128:(ti + 1) * 128], wo[:, hm, hf * 512:(hf + 1) * 512],
                           hm == 0, hm == MH - 1, [("oT", hm), ("wo",)], [PT(2 * po + hf)])
                stt("dve", r1[s4][:, :], acc[:, t, :], ALPHA, P[po][:, :], ALU.mult, ALU.add,
                    [("acc", t), PT(2 * po), PT(2 * po + 1)], [("r1", s4)])
                layer_norm_stats(r1[s4], ("r1", s4), st[s4], mv[s4], s4)
                act(xn[s4][:, :], r1[s4][:, :], AF.Identity, [("r1", s4), ("mv", s4)], [("xn", s4)],
                    bias=mv[s4][:, 3:4], scale=mv[s4][:, 2:3])
                act(acc[:, t, :], r1[s4][:, :], AF.Identity, [("r1", s4), ("mv", s4)], [("acc", t)],
                    bias=mv[s4][:, 3:4], scale=mv[s4][:, 2:3])
                tt("pool", acc[:, t, :], acc[:, t, :], lnbc[:, 0, :], ALU.mult, [("acc", t), ("lnbc", 4)], [("acc", t)])
                tt("pool", acc[:, t, :], acc[:, t, :], lnbc[:, 1, :], ALU.add, [("acc", t), ("lnbc", 4)], [("acc", t)])

        b2_qproj(0)
        for qb in range(NQB):
            b2_attn(qb)
            if qb >= 1:
                transposes_to_T(xn, mT, qb - 1, 4, 5, "xn", "mT")
            if qb + 1 < NQB:
                b2_qproj(qb + 1)
            b2_out_ln(qb)
        transposes_to_T(xn, mT, NQB - 1, 4, 5, "xn", "mT")
        A.release(mk)
        sc.barrier()
        if stage_i == 5:
            for t in range(NT):
                dma("sp", out_d[b, t * 128:(t + 1) * 128, :], acc[:, t, :], [("acc", t)], [("outdram", b, t)], "out")
            A.release(mk_b)
            sc.barrier()
            continue

        A.phase([R_tmp, R_x, R_y, R_hT])
        mk = A.mark()
        lnbc = A.alloc("lnbc", [128, 2, D], F32)
        load_lnbc(lnbc, 6)
        wr = A.alloc("wr", [128, KC, 36], BF16)
        brb = A.alloc("brb", [128, 36], F32)
        dma("pool", wr[:, :, :], wr_d.rearrange(WKC, p=128), [], [("wr",)], "wr")
        dma("sp", brb[:, :], br_d.partition_broadcast(128), [], [("brb",)], "brb")
        comb = A.alloc("comb", [128, NT, NE], F32)
        lg = A.alloc("lg", [128, 36], F32)
        cm = A.alloc("cm", [128, 8], F32)
        ec = A.alloc("ec", [128, 4], F32)
        oh = A.alloc("oh", [128, 4], F32)
        ef = A.alloc("ef", [128, 4, 8], F32)
        fm = A.alloc("fm", [128, 4], F32)
        v8 = A.alloc("v8", [128, 4, 8], F32)
        sg = A.alloc("sg", [128, 4], F32)
        msk = A.alloc("msk", [128, 4, 8], F32)
        def router(t):
            for kc in range(KC):
                mm(bank(5)[:, 0:36], mT[:, kc, t * 128:(t + 1) * 128], wr[:, kc, :], kc == 0, kc == KC - 1,
                   [("mT", kc, t // 4), ("wr",)], [PT(5)])
            tt("dve", lg[:, :], bank(5)[:, 0:36], brb[:, :], ALU.add, [PT(5), ("brb",)], [("lg",)])
            sc.add("dve", "tensor_reduce", dict(out=cm[:, 0:1], in_=lg[:, 0:4], axis=AX.X, op=ALU.max), [("lg",)], [("cm",)])
            ts("dve", oh[:, :], lg[:, 0:4], cm[:, 0:1], None, ALU.is_ge, None, [("lg",), ("cm",)], [("oh",)])
            ts("dve", cm[:, 1:2], cm[:, 0:1], -1.0, None, ALU.mult, None, [("cm",)], [("cm",)])
            act(ec[:, :], lg[:, 0:4], AF.Exp, [("lg",), ("cm",)], [("ec",)], bias=cm[:, 1:2], scale=1.0)
            sc.add("dve", "tensor_reduce", dict(out=cm[:, 2:3], in_=ec[:, :], axis=AX.X, op=ALU.add), [("ec",)], [("cm",)])
            sc.add("dve", "reciprocal", dict(out=cm[:, 3:4], in_=cm[:, 2:3]), [("cm",)], [("cm",)])
            lf = lg[:, 4:36].rearrange("p (g j) -> p g j", j=8)
            sc.add("dve", "tensor_reduce", dict(out=fm[:, :], in_=lf, axis=AX.X, op=ALU.max), [("lg",)], [("fm",)])
            tt("dve", ef[:, :, :], lf, fm[:, :].unsqueeze(2).to_broadcast([128, 4, 8]), ALU.subtract, [("lg",), ("fm",)], [("ef",)])
            act(ef[:, :, :], ef[:, :, :], AF.Exp, [("ef",)], [("ef",)])
            for g in range(4):
                sc.add("dve", "max", dict(out=v8[:, g, :], in_=ef[:, g, :]), [("ef",)], [("v8",)])
            ts("dve", sg[:, :], v8[:, :, 1], 1.0, None, ALU.add, None, [("v8",)], [("sg",)])
            sc.add("dve", "reciprocal", dict(out=sg[:, :], in_=sg[:, :]), [("sg",)], [("sg",)])
            stt("dve", sg[:, :], sg[:, :], cm[:, 3:4], oh[:, :], ALU.mult, ALU.mult, [("sg",), ("cm",), ("oh",)], [("sg",)])
            tt("dve", msk[:, :, :], ef[:, :, :], v8[:, :, 1:2].to_broadcast([128, 4, 8]), ALU.is_ge, [("ef",), ("v8",)], [("msk",)])
            tt("dve", msk[:, :, :], msk[:, :, :], ef[:, :, :], ALU.mult, [("msk",), ("ef",)], [("msk",)])
            tt("dve", comb[:, t, :].rearrange("p (g j) -> p g j", j=8), msk[:, :, :],
               sg[:, :].unsqueeze(2).to_broadcast([128, 4, 8]), ALU.mult, [("msk",), ("sg",)], [("comb", t)])
        wgu = [A.alloc("wgu", [128, 2, KC, 512], BF16) for _ in range(2)]
        wdn = [A.alloc("wdn", [128, 2, 2, D], BF16) for _ in range(2)]
        sa = [[A.alloc("sa", [128, 256], F32) for _ in range(2)] for _ in range(2)]
        hid = [[A.alloc("hid", [128, 256], BF16) for _ in range(2)] for _ in range(2)]
        hidT = [A.alloc("hidT", [128, 2, 2, 128], BF16) for _ in range(2)]
        NP_ = NE // 2
        NU = NP_ * NT
        pb4 = P[2][:, 0:512].bitcast(BF16)

        def w_dma(pr):
            ws = pr % 2
            for ei in range(2):
                e = pr * 2 + ei
                dma("pool", wgu[ws][:, ei, :, 0:256], wg_d[e].rearrange(WKC, p=128), [], [("wgu", ws)], f"wgu{ws}")
                dma("pool", wgu[ws][:, ei, :, 256:512], wu_d[e].rearrange(WKC, p=128), [], [("wgu", ws)], f"wgu{ws}")
                dma("pool", wdn[ws][:, ei, :, :], wd_d[e].rearrange("(c p) d -> p c d", p=128), [], [("wdn", ws)], f"wdn{ws}")

        def u_up(u):
            pr, t = divmod(u, NT)
            ws, par = pr % 2, u % 2
            for kc in range(KC):
                for ei in range(2):
                    mm(bank(2 * par + ei), mT[:, kc, t * 128:(t + 1) * 128], wgu[ws][:, ei, kc, :], kc == 0, kc == KC - 1,
                       [("mT", kc, t // 4), ("wgu", ws)], [PT(2 * par + ei)])

        def u_elem(u):
            pr, t = divmod(u, NT)
            par = u % 2
            for ei in range(2):
                e = pr * 2 + ei
                bk = 2 * par + ei
                act(sa[par][ei][:, :], bank(bk)[:, 0:256], AF.Silu, [PT(bk)], [("sa", par, ei)])
                stt("dve", hid[par][ei][:, :], sa[par][ei][:, :], comb[:, t, e:e + 1], bank(bk)[:, 256:512], ALU.mult, ALU.mult,
                    [("sa", par, ei), ("comb", t), PT(bk)], [("hid", par, ei)])

        def u_tr(u):
            par = u % 2
            for ei in range(2):
                for c in range(2):
                    col = par * 512 + (ei * 2 + c) * 128
                    tr(pb4[:, col:col + 128], hid[par][ei][:, c * 128:(c + 1) * 128], [("hid", par, ei)], [("ps4", par)])
            cp("act", hidT[par][:, :, :, :].rearrange("p e c t -> p (e c t)"), pb4[:, par * 512:(par + 1) * 512],
               [("ps4", par)], [("hidT", par)])

        def u_down(u):
            pr, t = divmod(u, NT)
            ws, par = pr % 2, u % 2
            for hf in range(2):
                k = 0
                for ei in range(2):
                    for c in range(2):
                        mm(P[3][:, hf * 512:(hf + 1) * 512], hidT[par][:, ei, c, :], wdn[ws][:, ei, c, hf * 512:(hf + 1) * 512],
                           k == 0, k == 3, [("hidT", par), ("wdn", ws)], [PT(6 + hf)])
                        k += 1
            if pr == 0:
                stt("dve", acc[:, t, :], acc[:, t, :], ALPHA, P[3][:, :], ALU.mult, ALU.add,
                    [("acc", t), PT(6), PT(7)], [("acc", t)])
            else:
                tt("dve", acc[:, t, :], acc[:, t, :], P[3][:, :], ALU.add, [("acc", t), PT(6), PT(7)], [("acc", t)])

        st3 = [A.alloc("st", [128, 2, 6], F32) for _ in range(2)]
        mv3 = [A.alloc("mv", [128, 4], F32) for _ in range(2)]
        ob_ = [A.alloc("ob", [128, D], F32) for _ in range(2)]

        def ln3_store(t):
            s2 = t % 2
            layer_norm_stats(acc[:, t, :], ("acc", t), st3[s2], mv3[s2], ("ln3", s2))
            act(ob_[s2][:, :], acc[:, t, :], AF.Identity, [("acc", t), ("mv", ("ln3", s2))], [("ob", s2)],
                bias=mv3[s2][:, 3:4], scale=mv3[s2][:, 2:3])
            tt("pool", ob_[s2][:, :], ob_[s2][:, :], lnbc[:, 0, :], ALU.mult, [("ob", s2), ("lnbc", 6)], [("ob", s2)])
            tt("pool", ob_[s2][:, :], ob_[s2][:, :], lnbc[:, 1, :], ALU.add, [("ob", s2), ("lnbc", 6)], [("ob", s2)])
            dma("sp", out_d[b, t * 128:(t + 1) * 128, :], ob_[s2][:, :], [("ob", s2)], [("outdram", b, t), ("ob", s2)], f"out{s2}")

        w_dma(0)
        w_dma(1)
        for t_ in range(3):
            router(t_)
        u_up(0)
        u_elem(0)
        for u in range(NU):
            pr, t = divmod(u, NT)
            if t == 0 and pr >= 1 and pr + 1 < NP_:
                w_dma(pr + 1)
            u_tr(u)
            if u + 1 < NU:
                u_up(u + 1)
                u_elem(u + 1)
            u_down(u)
            if pr == 0 and t + 3 < NT:
                router(t + 3)
            if pr == NP_ - 1:
                ln3_store(t)
        A.release(mk)
        A.release(mk_b)
        sc.barrier()

    fin = [("outdram", b, t) for b in range(nb) for t in range(NT)] + [("dbg", n) for n in dbg_out]
    sc.add("sp", "nop", dict(), fin, [])

    sc.finalize()
    keys = set()
    for op in sc.ops:
        for k, v in op.waits:
            keys.add(k)
        if op.dma_key is not None:
            keys.add(("dma", op.dma_key))
        elif op.sig:
            keys.add(("eng", op.eng))
    keys = sorted(keys)
    from contextlib import ExitStack
    with ExitStack() as es:
        sems = {k: es.enter_context(nc.semaphore(f"s_{k[0]}_{k[1]}")) for k in keys}
        block = es.enter_context(nc.Block())

        @block.tensor
        def _(e):
            sc.emit(nc, "pe", e, sems)

        @block.scalar
        def _(e):
            sc.emit(nc, "act", e, sems)

        @block.vector
        def _(e):
            sc.emit(nc, "dve", e, sems)

        @block.gpsimd
        def _(e):
            sc.emit(nc, "pool", e, sems)

        @block.sync
        def _(e):
            sc.emit(nc, "sp", e, sems)
    return nc, dbg_out, dict(n_ops=len(sc.ops), sbuf_peak=0, n_sems=len(keys))


def _rel_bucket_np(dist):
    d = np.maximum(dist, 0)
    large = 16 + (np.log(np.maximum(d, 1).astype(np.float32) / np.float32(16)) / np.float32(math.log(128 / 16))
                  * np.float32(16)).astype(np.int32)
    large = np.minimum(large, 31)
    return np.where(d < 16, d, large)


def make_inputs(inputs, nb=NB):
    f = lambda k: np.ascontiguousarray(np.asarray(inputs[k], dtype=np.float32))
    x = f("x")
    mem = f("mem")
    lnp = np.stack([f("ln_in_g").reshape(-1), f("ln_in_b").reshape(-1), f("ln1_g").reshape(-1), f("ln1_b").reshape(-1),
                    f("ln2_g").reshape(-1), f("ln2_b").reshape(-1), f("ln3_g").reshape(-1), f("ln3_b").reshape(-1)])
    rel = f("rel_bias")
    kk = np.arange(128)[:, None]
    qq = np.arange(128)[None, :]
    biasT = np.empty((H, 2, 128, 128), np.float32)
    for off in range(2):
        bk = _rel_bucket_np(off * 128 + qq - kk)
        biasT[:, off] = rel[bk].transpose(2, 0, 1)
    b31 = np.ascontiguousarray(np.broadcast_to(rel[31][None, :], (128, H)))
    maskd = (qq >= kk).astype(np.float32)
    tq = np.arange(NT)[:, None] // 2
    jj = np.arange(8)[None, :]
    pastbias = np.where(jj < tq, 0.0, -1e30).astype(np.float32)
    ownfix = np.where(jj == tq, 0.0, -1e9).astype(np.float32)
    pastbias = np.ascontiguousarray(np.broadcast_to(pastbias[None], (128, NT, 8)))
    ownfix = np.ascontiguousarray(np.broadcast_to(ownfix[None], (128, NT, 8)))
    ind = (np.arange(S)[None, :] // 256 == np.arange(8)[:, None]).astype(np.float32).astype(ml_dtypes.bfloat16)
    tt_ = np.arange(16)[None, :]
    ww = np.array([2, 4, 8, 16])[:, None]
    invc = (1.0 / np.minimum(tt_ + 1, ww)).astype(np.float32)
    invc = np.ascontiguousarray(np.broadcast_to(invc[None], (128, 4, 16)))
    shiftb = np.zeros((64, 128), np.float32)
    shiftb[np.arange(64), 64 + np.arange(64)] = 1.0
    common = {
        "lnp": lnp, "ident": np.eye(128, dtype=np.float32).astype(ml_dtypes.bfloat16), "identf": np.eye(128, dtype=np.float32),
        "shiftb": shiftb, "w_in": f("w_in")[0], "b_gate": f("b_gate")[0], "w_pool_grp": f("w_pool_grp")[0],
        "pool_scale": f("pool_scale")[0], "w_pool_up": f("w_pool_up")[0], "w_attn_up": f("w_attn_up")[0],
        "w_mix_out": f("w_mix_out")[0], "w_mq": f("w_mq")[0], "w_mk": f("w_mk")[0], "w_mv": f("w_mv")[0], "w_mo": f("w_mo")[0],
        "w_router": np.ascontiguousarray(np.concatenate([f("w_coarse")[0], f("w_fine")[0].reshape(D, 32)], axis=1)),
        "b_router": np.ascontiguousarray(np.concatenate([f("b_coarse")[0], f("b_fine")[0].reshape(32)])),
        "w_gate": f("w_gate")[0], "w_up": f("w_up")[0], "w_down": f("w_down")[0],
        "biasT": biasT, "b31": b31, "maskd": maskd, "pastbias": pastbias, "ownfix": ownfix, "indrows": ind, "invc": invc,
    }
    maps = []
    for c in range(NCORES):
        m = dict(common)
        m["x"] = x[c * nb:(c + 1) * nb]
        m["mem"] = mem[c * nb:(c + 1) * nb]
        maps.append(m)
    return maps


_CACHE = {}


def kernel(**inputs):
    if "nc" not in _CACHE:
        _CACHE["nc"] = build_program("all", debug=False)
    nc, _, _ = _CACHE["nc"]
    maps = make_inputs(inputs)
    res = run_bass_kernel_spmd(nc, maps, core_ids=list(range(NCORES)))
    outs = [np.asarray(r["out"]) for r in res.results]
    return np.concatenate(outs, axis=0).astype(np.float32)
```
